# Optimizing a Trainium2 kernel written in Bass

```python
import jax, jax.numpy as jnp
from jax import lax
import numpy as np


D_MODEL = 2048
BATCH = 4
SEQ = 2048
DEPTH = 4

EPS = 1e-6
LRU_HEADS = 12
LRU_HEAD_DIM = 128
LRU_WIDTH = LRU_HEADS * LRU_HEAD_DIM
CONV_WIDTH = 4
LRU_C = 8.0
DIL_PATTERNS = ((128, 1), (512, 4), (2048, 16))
N_DIL = 3
ATT_HEADS_PER_GROUP = 4
ATT_HEAD_DIM = 128
ATT_HEADS = N_DIL * ATT_HEADS_PER_GROUP
ATT_WIDTH = ATT_HEADS * ATT_HEAD_DIM
ATT_OUT = ATT_HEADS_PER_GROUP * ATT_HEAD_DIM
EVEN_SIZES = (LRU_WIDTH, LRU_WIDTH, ATT_WIDTH, ATT_WIDTH, ATT_WIDTH, ATT_OUT)
EVEN_IN = 2 * LRU_WIDTH + 3 * ATT_WIDTH + ATT_OUT
EVEN_OUT = LRU_WIDTH + ATT_OUT
GLA_HEADS = 4
GLA_DK = 128
GLA_DV = 256
GLA_KW = GLA_HEADS * GLA_DK
GLA_VW = GLA_HEADS * GLA_DV
GLA_RANK = 16
GLA_TAU = 16.0
GLA_CHUNK = 64
SG_GROUPS = 4
SG_GROUP_DIM = 256
SG_WIDTH = SG_GROUPS * SG_GROUP_DIM
SG_CHUNK = 128
ODD_SIZES = (GLA_KW, GLA_KW, GLA_VW, GLA_RANK, GLA_VW, SG_WIDTH, SG_WIDTH, SG_WIDTH)
ODD_IN = 2 * GLA_KW + 2 * GLA_VW + GLA_RANK + 3 * SG_WIDTH
ODD_OUT = GLA_VW + SG_WIDTH

kernel_name = 'hybrid_lru_dilattn_gla_sgu_trunk'

F32 = jnp.float32


def split_points(sizes):
    pts, acc = [], 0
    for s in sizes[:-1]:
        acc += s
        pts.append(acc)
    return pts


def rms_norm(x, g):
    xf = x.astype(F32)
    y = xf * lax.rsqrt(jnp.mean(xf * xf, axis=-1, keepdims=True) + EPS)
    return (y * g.astype(F32)).astype(x.dtype)


def causal_conv(x, w, b):
    k_w = w.shape[0]
    seq = x.shape[1]
    xp = jnp.pad(x, ((0, 0), (k_w - 1, 0), (0, 0)))
    return sum(xp[:, j:j + seq] * w[j] for j in range(k_w)) + b


def _lru_combine(left, right):
    a_l, h_l = left
    a_r, h_r = right
    return a_l * a_r, a_r * h_l + h_r


def rg_lru(x, w_a, b_a, w_i, b_i, lam):
    bsz, seq, width = x.shape
    xf = x.astype(F32)
    xh = xf.reshape(bsz, seq, LRU_HEADS, LRU_HEAD_DIM)
    r = jax.nn.sigmoid(jnp.einsum('bshi,hij->bshj', xh, w_a.astype(F32)) + b_a.astype(F32))
    i = jax.nn.sigmoid(jnp.einsum('bshi,hij->bshj', xh, w_i.astype(F32)) + b_i.astype(F32))
    r = r.reshape(bsz, seq, width)
    i = i.reshape(bsz, seq, width)
    log_a = -LRU_C * r * jax.nn.softplus(-lam.astype(F32))
    a = jnp.exp(log_a)
    u = jnp.sqrt(-jnp.expm1(2.0 * log_a)) * (i * xf)
    _, hs = lax.associative_scan(_lru_combine, (a, u), axis=1)
    return hs.astype(x.dtype)


def alibi_slopes():
    g = np.arange(N_DIL)[:, None]
    j = np.arange(ATT_HEADS_PER_GROUP)[None, :]
    return jnp.asarray(2.0 ** (-8.0 * (g + N_DIL * j + 1) / ATT_HEADS), F32)


def dilated_group(q, k, v, dil, sub_w, slopes):
    bsz, seq, nh, dh = q.shape
    L = seq // dil
    nb = -(-L // sub_w)
    Lp = nb * sub_w

    def strided(t):
        t = t.reshape(bsz, L, dil, nh, dh).transpose(0, 2, 3, 1, 4)
        return jnp.pad(t, ((0, 0), (0, 0), (0, 0), (0, Lp - L), (0, 0)))

    def banded(t):
        t = jnp.pad(t, ((0, 0), (0, 0), (0, 0), (sub_w, 0), (0, 0)))
        t = t.reshape(bsz, dil, nh, nb + 1, sub_w, dh)
        return jnp.concatenate([t[:, :, :, :-1], t[:, :, :, 1:]], axis=-2)

    qb = strided(q).reshape(bsz, dil, nh, nb, sub_w, dh)
    kb = banded(strided(k))
    vb = banded(strided(v))
    s = jnp.einsum('brhnqe,brhnke->brhnqk', qb, kb) * (dh ** -0.5)
    qi = jnp.arange(sub_w)[:, None]
    ki = jnp.arange(2 * sub_w)[None, :]
    delta = sub_w + qi - ki
    blk = jnp.arange(nb)[:, None, None]
    valid = (delta >= 0) & (delta <= sub_w) & (blk * sub_w - sub_w + ki >= 0)
    dist = (delta * dil).astype(F32)
    s = s - slopes[None, None, :, None, None, None] * dist
    s = jnp.where(valid, s, -jnp.inf)
    m = jnp.max(s, axis=-1, keepdims=True)
    p = jnp.exp(s - m)
    den = jnp.sum(p, axis=-1, keepdims=True)
    o = jnp.einsum('brhnqk,brhnke->brhnqe', p, vb) / den
    lse = (m + jnp.log(den))[..., 0]

    def unstrided(t):
        t = t.reshape(bsz, dil, nh, Lp, *t.shape[5:])[:, :, :, :L]
        t = jnp.moveaxis(t, 3, 1)
        return t.reshape(bsz, seq, nh, *t.shape[4:])

    return unstrided(o), unstrided(lse)


def dilated_attention(q, k, v):
    bsz, seq, _ = q.shape
    shape = (bsz, seq, N_DIL, ATT_HEADS_PER_GROUP, ATT_HEAD_DIM)
    qh, kh, vh = (t.astype(F32).reshape(shape) for t in (q, k, v))
    slopes = alibi_slopes()
    outs, lses = [], []
    for g, (win, dil) in enumerate(DIL_PATTERNS):
        o, l = dilated_group(qh[:, :, g], kh[:, :, g], vh[:, :, g], dil, win // dil, slopes[g])
        outs.append(o)
        lses.append(l)
    wts = jax.nn.softmax(jnp.stack(lses, axis=0), axis=0)
    y = jnp.einsum('gbsh,gbshe->bshe', wts, jnp.stack(outs, axis=0))
    return y.reshape(bsz, seq, ATT_OUT).astype(q.dtype)


def gla_chunked(q, k, v, log_a):
    bsz, seq, nh, dk = q.shape
    dv = v.shape[-1]
    nc = seq // GLA_CHUNK

    def chunks(t):
        return t.astype(F32).reshape(bsz, nc, GLA_CHUNK, nh, t.shape[-1]).transpose(0, 3, 1, 2, 4)

    q, k, v, log_a = chunks(q), chunks(k), chunks(v), chunks(log_a)
    q = q * (dk ** -0.5)
    b = jnp.cumsum(log_a, axis=3)
    b_last = b[:, :, :, -1:, :]
    qt = q * jnp.exp(b)
    kt = k * jnp.exp(-b)
    causal = jnp.tril(jnp.ones((GLA_CHUNK, GLA_CHUNK), bool))
    att = jnp.where(causal, jnp.einsum('bhnie,bhnje->bhnij', qt, kt), 0.0)
    o_intra = jnp.einsum('bhnij,bhnjv->bhniv', att, v)
    upd = jnp.einsum('bhnje,bhnjv->bhnev', k * jnp.exp(b_last - b), v)
    decay = jnp.exp(b_last[:, :, :, 0, :])

    def step(state, inp):
        dec, u = inp
        return dec[..., None] * state + u, state

    init = jnp.zeros((bsz, nh, dk, dv), F32)
    _, s_prev = lax.scan(step, init, (jnp.moveaxis(decay, 2, 0), jnp.moveaxis(upd, 2, 0)))
    o_inter = jnp.einsum('bhnie,nbhev->bhniv', qt, s_prev)
    o = o_intra + o_inter
    return o.transpose(0, 2, 3, 1, 4).reshape(bsz, seq, nh, dv)


def spatial_gating(u, v, w_s, b_s, v_norm):
    bsz, seq, _ = u.shape
    nc = seq // SG_CHUNK
    vf = v.astype(F32).reshape(bsz, nc, SG_CHUNK, SG_GROUPS, SG_GROUP_DIM)
    mu = jnp.mean(vf, axis=-1, keepdims=True)
    var = jnp.mean(jnp.square(vf - mu), axis=-1, keepdims=True)
    vf = (vf - mu) * lax.rsqrt(var + EPS) * v_norm.astype(F32)
    mask = jnp.tril(jnp.ones((SG_CHUNK, SG_CHUNK), bool))
    w = jnp.where(mask, w_s.astype(F32), 0.0)
    s = jnp.einsum('gts,bcsge->bctge', w, vf) + b_s.astype(F32).T[:, :, None]
    return (u.astype(F32) * s.reshape(bsz, seq, SG_WIDTH)).astype(u.dtype)


def even_layer(x, norm, w_in, conv_w, conv_b, w_a, b_a, w_i, b_i, lam, w_out):
    h = rms_norm(x, norm)
    z = h @ w_in
    xa, ga, q, k, v, gb = jnp.split(z, split_points(EVEN_SIZES), axis=-1)
    ya = rg_lru(causal_conv(xa, conv_w, conv_b), w_a, b_a, w_i, b_i, lam) * jax.nn.silu(ga)
    yb = dilated_attention(q, k, v) * jax.nn.silu(gb)
    return jnp.concatenate([ya.astype(x.dtype), yb.astype(x.dtype)], axis=-1) @ w_out


def odd_layer(x, norm, w_in, w_alpha, b_alpha, head_norm, v_norm, w_s, b_s, w_out):
    bsz, seq, _ = x.shape
    h = rms_norm(x, norm)
    z = h @ w_in
    q, k, v, a_lr, gc, u, vd, gd = jnp.split(z, split_points(ODD_SIZES), axis=-1)
    log_a = jax.nn.log_sigmoid((a_lr @ w_alpha + b_alpha).astype(F32)) / GLA_TAU

    def heads(t, e):
        return t.reshape(bsz, seq, GLA_HEADS, e)

    o = gla_chunked(heads(q, GLA_DK), heads(k, GLA_DK), heads(v, GLA_DV), heads(log_a, GLA_DK))
    yc = rms_norm(o, head_norm).reshape(bsz, seq, GLA_VW).astype(x.dtype) * jax.nn.silu(gc)
    yd = spatial_gating(jax.nn.gelu(u), jax.nn.gelu(vd), w_s, b_s, v_norm) * jax.nn.silu(gd)
    return jnp.concatenate([yc.astype(x.dtype), yd.astype(x.dtype)], axis=-1) @ w_out


def setup_inputs(seed: int = 0) -> dict:
    key = jax.random.key(seed)
    ks = jax.random.split(key, 21)
    ne, no = (DEPTH + 1) // 2, DEPTH // 2

    def nrm(k, shape, scale):
        return scale * jax.random.normal(k, shape, F32)

    def gain(k, shape):
        return 1.0 + 0.02 * jax.random.normal(k, shape, F32)

    s_lru = jax.random.uniform(ks[9], (ne, LRU_WIDTH), F32, minval=0.9, maxval=0.999) ** (1.0 / LRU_C)
    return {
        'x': nrm(ks[0], (BATCH, SEQ, D_MODEL), 1.0),
        'ev_norm': gain(ks[1], (ne, D_MODEL)),
        'ev_w_in': nrm(ks[2], (ne, D_MODEL, EVEN_IN), D_MODEL ** -0.5),
        'ev_conv_w': nrm(ks[3], (ne, CONV_WIDTH, LRU_WIDTH), CONV_WIDTH ** -0.5),
        'ev_conv_b': nrm(ks[4], (ne, LRU_WIDTH), 0.01),
        'ev_w_a': nrm(ks[5], (ne, LRU_HEADS, LRU_HEAD_DIM, LRU_HEAD_DIM), LRU_HEAD_DIM ** -0.5),
        'ev_b_a': nrm(ks[6], (ne, LRU_HEADS, LRU_HEAD_DIM), 0.01),
        'ev_w_i': nrm(ks[7], (ne, LRU_HEADS, LRU_HEAD_DIM, LRU_HEAD_DIM), LRU_HEAD_DIM ** -0.5),
        'ev_b_i': nrm(ks[8], (ne, LRU_HEADS, LRU_HEAD_DIM), 0.01),
        'ev_lam': jnp.log(s_lru) - jnp.log1p(-s_lru),
        'ev_w_out': nrm(ks[10], (ne, EVEN_OUT, D_MODEL), EVEN_OUT ** -0.5),
        'od_norm': gain(ks[11], (no, D_MODEL)),
        'od_w_in': nrm(ks[12], (no, D_MODEL, ODD_IN), D_MODEL ** -0.5),
        'od_w_alpha': nrm(ks[13], (no, GLA_RANK, GLA_KW), GLA_RANK ** -0.5),
        'od_b_alpha': nrm(ks[14], (no, GLA_KW), 0.01),
        'od_head_norm': gain(ks[15], (no, GLA_HEADS, GLA_DV)),
        'od_v_norm': gain(ks[16], (no, SG_GROUPS, SG_GROUP_DIM)),
        'od_w_s': nrm(ks[17], (no, SG_GROUPS, SG_CHUNK, SG_CHUNK), SG_CHUNK ** -0.5),
        'od_b_s': 1.0 + nrm(ks[18], (no, SG_GROUPS, SG_CHUNK), 0.01),
        'od_w_out': nrm(ks[19], (no, ODD_OUT, D_MODEL), ODD_OUT ** -0.5),
        'final_norm': gain(ks[20], (D_MODEL,)),
    }


def reference(x, ev_norm, ev_w_in, ev_conv_w, ev_conv_b, ev_w_a, ev_b_a, ev_w_i, ev_b_i, ev_lam,
              ev_w_out, od_norm, od_w_in, od_w_alpha, od_b_alpha, od_head_norm, od_v_norm,
              od_w_s, od_b_s, od_w_out, final_norm):
    for layer in range(DEPTH):
        i = layer // 2
        if layer % 2 == 0:
            x = x + even_layer(x, ev_norm[i], ev_w_in[i], ev_conv_w[i], ev_conv_b[i], ev_w_a[i],
                               ev_b_a[i], ev_w_i[i], ev_b_i[i], ev_lam[i], ev_w_out[i])
        else:
            x = x + odd_layer(x, od_norm[i], od_w_in[i], od_w_alpha[i], od_b_alpha[i],
                              od_head_norm[i], od_v_norm[i], od_w_s[i], od_b_s[i], od_w_out[i])
    return rms_norm(x, final_norm)
```

```python
import numpy as np
import concourse.bass as bass
import concourse.mybir as mybir
from concourse.bass_utils import run_bass_kernel_spmd

F32 = mybir.dt.float32
BF16 = mybir.dt.bfloat16
AF = mybir.ActivationFunctionType
ALU = mybir.AluOpType

D = 2048
T = 2048
NCH = 16
EPS = 1e-6
SEM_LIMIT = 16000
SAME_ENG_SYNC = True
N_CORES = 8

DIL = (1, 4, 16)


class Tk:
    __slots__ = ("name", "w", "r", "dsem", "dcnt", "t")

    def __init__(self, name, t=None):
        self.name = name
        self.w = None
        self.r = {}
        self.dsem = None
        self.dcnt = 0
        self.t = t


class Eng:
    def __init__(self, name, h):
        self.name = name
        self.h = h
        self.sem = None
        self.cnt = 0
        self.known = {}
        self.nsem = 0


class Prog:
    def __init__(self):
        self.nc = bass.Bass("TRN2", target_bir_lowering=False)
        nc = self.nc
        self.E = {
            "pe": Eng("pe", nc.tensor),
            "act": Eng("act", nc.scalar),
            "dve": Eng("dve", nc.vector),
            "pool": Eng("pool", nc.gpsimd),
            "sp": Eng("sp", nc.sync),
        }
        self.n_inst = 0
        self.n_wait = 0
        self.dma_tks = {}
        self.allsems = []
        self.sem_pool = []
        self.swdge_sems = set()

    def sb(self, name, shape, dt):
        t = self.nc.alloc_sbuf_tensor(name, list(shape), dt)
        return Tk(name, t)

    def ps(self, name):
        t = self.nc.alloc_psum_tensor(name, [128, 512], F32)
        return Tk(name, t)

    def dram(self, name, shape, dt, kind):
        t = self.nc.dram_tensor(name, list(shape), dt, kind=kind)
        return Tk(name, t.ap())

    def _wait(self, e, evs):
        for (sem, val) in evs:
            k = id(sem)
            if e.known.get(k, 0) >= val:
                continue
            e.h.wait_ge(sem, val)
            e.known[k] = val
            self.n_wait += 1

    def _deps(self, e, reads, writes):
        evs = {}

        def add(ev):
            if ev is None:
                return
            k = id(ev[0])
            if k not in evs or evs[k][1] < ev[1]:
                evs[k] = ev

        for t in reads:
            add(t.w)
        for t in writes:
            add(t.w)
            for ev in t.r.values():
                add(ev)
        out = []
        for ev in evs.values():
            if ev[0] is e.sem:
                if e.name == "pe" or (e.name != "pool" and not SAME_ENG_SYNC):
                    continue
            out.append(ev)
        return out

    def _newsem(self, e):
        if e.sem is None or e.cnt >= SEM_LIMIT:
            e.sem = self.nc.alloc_semaphore(f"s_{e.name}_{e.nsem}")
            self.allsems.append(e.sem)
            e.nsem += 1
            e.cnt = 0

    def op(self, en, fn, reads=(), writes=()):
        e = self.E[en]
        self._newsem(e)
        self._wait(e, self._deps(e, reads, writes))
        inst = fn(e.h)
        e.cnt += 1
        inst.then_inc(e.sem, 1)
        ev = (e.sem, e.cnt)
        for t in reads:
            t.r[id(e.sem)] = ev
        for t in writes:
            t.w = ev
            t.r = {}
        self.n_inst += 1
        return inst

    def dma(self, qn, out, in_, dst, semtk, reads=(), **kw):
        e = self.E[qn]
        if semtk.dsem is None:
            if self.sem_pool and qn != "pool":
                semtk.dsem, semtk.dcnt = self.sem_pool.pop()
            else:
                semtk.dsem = self.nc.alloc_semaphore(f"d{len(self.allsems)}_{semtk.name}")
                self.allsems.append(semtk.dsem)
        if qn == "pool":
            self.swdge_sems.add(id(semtk.dsem))
        self.dma_tks[id(semtk)] = semtk
        self._wait(e, self._deps(e, reads, (dst,)))
        inst = e.h.dma_start(out=out, in_=in_, **kw)
        semtk.dcnt += 16
        inst.then_inc(semtk.dsem, 16)
        ev = (semtk.dsem, semtk.dcnt)
        for t in reads:
            t.r[id(semtk.dsem)] = ev
        dst.w = ev
        dst.r = {}
        self.n_inst += 1
        return inst

    def wait_all(self, en, tks):
        e = self.E[en]
        evs = []
        for t in tks:
            if t.w is not None:
                evs.append(t.w)
        self._wait(e, evs)

    def barrier(self):
        evs = []
        for f in self.E.values():
            if f.sem is not None and f.cnt > 0:
                evs.append((f.sem, f.cnt))
        for t in self.dma_tks.values():
            if t.dcnt > 0:
                evs.append((t.dsem, t.dcnt))
        for e in self.E.values():
            self._wait(e, evs)


class Builder:
    def __init__(self, layers, final=True):
        self.P = Prog()
        P = self.P
        self.layers = layers
        self.n_lru = 12
        self.final = final
        nc = P.nc
        self.nc = nc
        self.xin = P.dram("xin", [NCH, 128, T], F32, "ExternalInput")
        self.yout = P.dram("yout", [NCH, 128, T], F32, "ExternalOutput")
        self.youts = [[Tk(f"yo{c}_{g}") for g in range(4)] for c in range(NCH)]
        self.xs_ap = nc.dram_tensor("xs", [NCH, 128, T], F32, kind="Internal").ap()
        self.xs = [[Tk(f"xs{c}_{g}") for g in range(4)] for c in range(NCH)]
        self.cst_d = P.dram("cst", [128, 4 * 128], F32, "ExternalInput")
        self.amask_d = P.dram("amask", [4, 128, 3 * 256], F32, "ExternalInput")
        self.fg_d = P.dram("fg", [128, NCH], F32, "ExternalInput")
        self.Ld = []
        for li, kind in enumerate(layers):
            d = {}
            if kind == "e":
                d["win"] = P.dram(f"L{li}_win", [64, 128, 2048], F32, "ExternalInput")
                d["wout"] = P.dram(f"L{li}_wout", [16, 128, 2048], F32, "ExternalInput")
                d["sp"] = P.dram(f"L{li}_sp", [128, 112], F32, "ExternalInput")
                d["wg"] = P.dram(f"L{li}_wg", [128, 2 * 12 * 128], F32, "ExternalInput")
            else:
                d["win"] = P.dram(f"L{li}_win", [48, 128, 2048], F32, "ExternalInput")
                d["wina"] = P.dram(f"L{li}_wina", [128, 256], F32, "ExternalInput")
                d["wout"] = P.dram(f"L{li}_wout", [16, 128, 2048], F32, "ExternalInput")
                d["sp"] = P.dram(f"L{li}_sp", [128, 32], F32, "ExternalInput")
                d["wal"] = P.dram(f"L{li}_wal", [17, 512], F32, "ExternalInput")
                d["wsT"] = P.dram(f"L{li}_wsT", [128, 512], F32, "ExternalInput")
                d["bsb"] = P.dram(f"L{li}_bsb", [128, 512], F32, "ExternalInput")
            self.Ld.append(d)

        self.hT_t = nc.alloc_sbuf_tensor("hT", [128, NCH, T], BF16)
        self.hT = [Tk(f"hT{c}", self.hT_t) for c in range(NCH)]
        self.yT_t = nc.alloc_sbuf_tensor("yT", [128, NCH, T], BF16)
        self.yT = [Tk(f"yT{c}", self.yT_t) for c in range(NCH)]
        self.yflat = self.yT_t[:, :, :].rearrange("p c t -> p (c t)")
        self.NSLOT = 6
        self.wslot = [P.sb(f"wslot{i}", [128, NCH * 128], BF16) for i in range(self.NSLOT)]
        self.wnext = 0
        self.ones = P.sb("ones", [128, 128], BF16)
        self.cst = P.sb("cstsb", [128, 512], F32)
        self.spar = P.sb("spar", [128, 112], F32)
        self.sparn = P.sb("sparn", [128, 16], F32)
        self.fg = P.sb("fgsb", [128, NCH], F32)
        self.epsb = P.sb("epsb", [128, 1], F32)
        self.oneb = P.sb("oneb", [128, 1], F32)
        self.banks = [P.ps(f"bank{i}") for i in range(8)]
        self._wk = {}
        self._guards = []
        self._ptks = []
        self._pid = 0

        P.op("dve", lambda h: h.memset(self.ones.t[:], 1.0), writes=[self.ones])
        P.op("dve", lambda h: h.memset(self.epsb.t[:], EPS), writes=[self.epsb])
        P.op("dve", lambda h: h.memset(self.oneb.t[:], 1.0), writes=[self.oneb])
        P.dma("sp", self.cst.t[:], self.cst_d.t[:, :], self.cst, self.cst)
        P.dma("sp", self.fg.t[:], self.fg_d.t[:, :], self.fg, self.fg)

    def palloc(self, name, shape, dt):
        g = self.nc.sbuf_tensor(f"{name}_p{self._pid}", list(shape), dt)
        t = g.__enter__()
        self._guards.append(g)
        tk = Tk(name, t)
        self._ptks.append(tk)
        return tk

    def end_phase(self):
        self.P.barrier()
        for g in reversed(self._guards):
            g.__exit__(None, None, None)
        self._guards = []
        for tk in self._ptks:
            if tk.dsem is not None:
                if id(tk.dsem) not in self.P.swdge_sems:
                    self.P.sem_pool.append((tk.dsem, tk.dcnt))
                self.P.dma_tks.pop(id(tk), None)
                tk.dsem = None
        self._ptks = []
        self._wk = {}
        self._pid += 1

    def wk(self, name, shape, dt, n=2):
        if name not in self._wk:
            self._wk[name] = [[self.palloc(f"{name}_{i}", shape, dt) for i in range(n)], 0]
        lst = self._wk[name]
        t = lst[0][lst[1] % n]
        lst[1] += 1
        return t

    def load_w(self, src_ap):
        P = self.P
        s = self.wslot[self.wnext % self.NSLOT]
        self.wnext += 1
        P.dma("pool", s.t[:, :], src_ap, s, s)
        return s

    def proj_fm(self, ws, tg, bank, nrow=128):
        P = self.P
        for kc in range(NCH):
            P.op("pe", lambda h: h.matmul(
                bank.t[0:nrow, :], ws.t[:, kc * nrow:(kc + 1) * nrow], self.hT_t[:, kc, tg * 512:(tg + 1) * 512],
                start=(kc == 0), stop=(kc == NCH - 1)),
                reads=[ws, self.hT[kc]], writes=[bank])

    def proj_tm(self, ws, bank, osl, tsl):
        P = self.P
        for kc in range(NCH):
            P.op("pe", lambda h: h.matmul(
                bank.t[:, osl], self.hT_t[:, kc, tsl], ws.t[:, kc * 128:(kc + 1) * 128],
                start=(kc == 0 and self._tm_first), stop=(kc == NCH - 1 and self._tm_last)),
                reads=[ws, self.hT[kc]], writes=[bank])

    def norm_stats_step(self, xt, c, tg, ssq, gain_tk, write_h, defer=False):
        P = self.P
        sl = slice(tg * 512, (tg + 1) * 512)
        sq = self.wk("sq", [128, 512], BF16, n=3)
        P.op("act", lambda h: h.activation(out=sq.t[:], in_=xt.t[:], func=AF.Square), reads=[xt], writes=[sq])

        def emit_ssq():
            P.op("pe", lambda h: h.matmul(ssq[tg].t[:], self.ones.t[:], sq.t[:], start=(c == 0), stop=(c == NCH - 1)),
                 reads=[self.ones, sq], writes=[ssq[tg]])

        if write_h:
            P.op("act", lambda h: h.activation(
                out=self.hT_t[:, c, sl], in_=xt.t[:], func=AF.Copy, scale=gain_tk.t[:, c:c + 1]),
                reads=[xt, gain_tk], writes=[self.hT[c]])
        if defer:
            return emit_ssq
        emit_ssq()
        return None

    def first_norm(self):
        P = self.P
        ssq = self.banks[4:8]
        for c in range(NCH):
            for tg in range(4):
                sl = slice(tg * 512, (tg + 1) * 512)
                xt = self.wk("xt", [128, 512], F32, n=3)
                P.dma("sp", xt.t[:], self.xin.t[c, :, sl], xt, xt)
                self.norm_stats_step(xt, c, tg, ssq, self.spar, True)
                P.dma("sp", self.xs_ap[c, :, sl], xt.t[:], self.xs[c][tg], xt, reads=[xt])
        self.finish_norm(ssq)
        self.scale_h()
        self.end_phase()

    def finish_norm(self, ssq):
        P = self.P
        self.rstd = self.palloc("rstd", [128, T], F32)
        for tg in range(4):
            sl = slice(tg * 512, (tg + 1) * 512)
            tmp = self.wk("nrm_tmp", [128, 512], F32)
            P.op("act", lambda h: h.activation(out=tmp.t[:], in_=ssq[tg].t[:], func=AF.Sqrt,
                                               scale=1.0 / D, bias=self.epsb.t[:, 0:1]),
                 reads=[ssq[tg], self.epsb], writes=[tmp])
            P.op("dve", lambda h: h.reciprocal(out=self.rstd.t[:, sl], in_=tmp.t[:]),
                 reads=[tmp], writes=[self.rstd])

    def scale_h(self):
        P = self.P
        for c in range(NCH):
            P.op("dve", lambda h: h.tensor_tensor(out=self.hT_t[:, c, :], in0=self.hT_t[:, c, :],
                                               in1=self.rstd.t[:], op=ALU.mult),
                 reads=[self.rstd], writes=[self.hT[c]])

    def out_phase(self, li, last):
        P = self.P
        d = self.Ld[li]
        ssq = self.banks[4:8]
        if not last:
            P.dma("sp", self.sparn.t[:, 0:16], self.Ld[li + 1]["sp"].t[:, 0:16], self.sparn, self.sparn)
        pending = None
        for dc in range(NCH):
            ws = self.load_w(d["wout"].t[dc, :, :])
            for tg in range(4):
                sl = slice(tg * 512, (tg + 1) * 512)
                xt = self.wk("xt", [128, 512], F32, n=4)
                P.dma("sp", xt.t[:], self.xs_ap[dc, :, sl], xt, xt, reads=[self.xs[dc][tg]])
                bank = self.banks[(dc * 4 + tg) % 2]
                for fc in range(NCH):
                    P.op("pe", lambda h: h.matmul(
                        bank.t[:], ws.t[:, fc * 128:(fc + 1) * 128], self.yT_t[:, fc, sl],
                        start=(fc == 0), stop=(fc == NCH - 1)),
                        reads=[ws, self.yT[fc]], writes=[bank])
                if pending is not None:
                    pending()
                P.op("dve", lambda h: h.tensor_tensor(out=xt.t[:], in0=bank.t[:], in1=xt.t[:], op=ALU.add),
                     reads=[bank], writes=[xt])
                pending = self.norm_stats_step(xt, dc, tg, ssq, self.sparn, not last, defer=True)
                P.dma("sp", self.xs_ap[dc, :, sl], xt.t[:], self.xs[dc][tg], xt, reads=[xt])
        if pending is not None:
            pending()
        self.finish_norm(ssq)
        if not last:
            self.scale_h()
        else:
            self.final_out()
        self.end_phase()

    def final_out(self):
        P = self.P
        for c in range(NCH):
            for tg in range(4):
                sl = slice(tg * 512, (tg + 1) * 512)
                xt = self.wk("xt", [128, 512], F32, n=3)
                P.dma("sp", xt.t[:], self.xs_ap[c, :, sl], xt, xt, reads=[self.xs[c][tg]])
                if self.final:
                    P.op("dve", lambda h: h.scalar_tensor_tensor(
                        out=xt.t[:], in0=xt.t[:], scalar=self.fg.t[:, c:c + 1], in1=self.rstd.t[:, sl],
                        op0=ALU.mult, op1=ALU.mult),
                        reads=[self.fg, self.rstd], writes=[xt])
                P.dma("sp", self.yout.t[c, :, sl], xt.t[:], self.youts[c][tg], xt, reads=[xt])
        P.wait_all("sp", [self.youts[c][tg] for c in range(NCH) for tg in range(4)])

    def load_params(self, li):
        P = self.P
        d = self.Ld[li]
        n = 112 if self.layers[li] == "e" else 32
        P.dma("sp", self.spar.t[:, 0:n], d["sp"].t[:, :], self.spar, self.spar)

    def even_layer(self, li):
        self.attention(li)
        self.end_phase()
        self.lru(li)
        self.end_phase()

    def lru(self, li):
        P = self.P
        d = self.Ld[li]
        sp = self.spar
        wg = self.palloc("wg", [128, 2 * 12 * 128], BF16)
        for q in range(2):
            P.dma("pool", wg.t[:, q * 1536:(q + 1) * 1536], d["wg"].t[:, q * 1536:(q + 1) * 1536], wg, wg)
        cc = self.palloc("lru_c", [128, 12], F32)
        ee = self.palloc("lru_e", [128, 12], F32)
        t1 = self.palloc("lru_t1", [128, 12], F32)
        P.op("act", lambda h: h.activation(out=ee.t[:], in_=sp.t[:, 76:88], func=AF.Exp, scale=-1.0),
             reads=[sp], writes=[ee])
        P.op("dve", lambda h: h.tensor_scalar(out=t1.t[:], in0=ee.t[:], scalar1=-1.0 / 3.0, scalar2=0.5,
                                              op0=ALU.mult, op1=ALU.add), reads=[ee], writes=[t1])
        P.op("dve", lambda h: h.tensor_tensor(out=t1.t[:], in0=t1.t[:], in1=ee.t[:], op=ALU.mult),
             reads=[ee], writes=[t1])
        P.op("dve", lambda h: h.tensor_scalar(out=t1.t[:], in0=t1.t[:], scalar1=-1.0, scalar2=1.0,
                                              op0=ALU.mult, op1=ALU.add), reads=[], writes=[t1])
        P.op("dve", lambda h: h.tensor_tensor(out=t1.t[:], in0=t1.t[:], in1=ee.t[:], op=ALU.mult),
             reads=[ee], writes=[t1])
        P.op("dve", lambda h: h.tensor_scalar(out=cc.t[:], in0=t1.t[:], scalar1=-8.0, scalar2=None, op0=ALU.mult),
             reads=[t1], writes=[cc])
        hbias = self.palloc("lru_hb", [128, 36], F32)
        six = self.palloc("lru_six", [128, 1], F32)
        P.op("dve", lambda h: h.tensor_scalar(out=hbias.t[:, 0:24], in0=sp.t[:, 88:112], scalar1=0.5, scalar2=None,
                                              op0=ALU.mult), reads=[sp], writes=[hbias])
        P.op("dve", lambda h: h.tensor_scalar(out=hbias.t[:, 24:36], in0=cc.t[:], scalar1=0.5, scalar2=None,
                                              op0=ALU.mult), reads=[cc], writes=[hbias])
        P.op("dve", lambda h: h.memset(six.t[:], 1.0 / 16.0), writes=[six])
        NH = self.n_lru
        steps = [(hh, tg) for hh in range(NH) for tg in range(4)]
        wts = {}
        xas = {}
        hprev = {}

        def stage_a(si):
            hh, tg = steps[si]
            if tg == 0:
                wts[hh] = (self.load_w(d["win"].t[hh, :, :]), self.load_w(d["win"].t[NH + hh, :, :]))
                xas[hh] = self.wk("lru_xa", [128, 4, 3 + 512], F32, n=2)
            w_xa, w_ga = wts[hh]
            xa = xas[hh]
            b0 = self.banks[0 + si % 2]
            b1 = self.banks[4 + si % 2]
            self.proj_fm(w_xa, tg, b0)
            self.proj_fm(w_ga, tg, b1)
            P.op("act", lambda h: h.activation(out=xa.t[:, tg, 3:515], in_=b0.t[:], func=AF.Copy),
                 reads=[b0], writes=[xa])
            if tg == 0:
                P.op("dve", lambda h: h.memset(xa.t[:, 0, 0:3], 0.0), writes=[xa])
            else:
                P.op("dve", lambda h: h.tensor_copy(out=xa.t[:, tg, 0:3], in_=xa.t[:, tg - 1, 512:515]),
                     writes=[xa])
            sgt = self.wk("lru_sgt", [128, 512], F32, n=1)
            P.op("act", lambda h: h.activation(out=sgt.t[:], in_=b1.t[:], func=AF.Tanh, scale=0.5),
                 reads=[b1], writes=[sgt])
            sg = self.wk("lru_sg", [128, 512], BF16, n=3)
            P.op("dve", lambda h: h.scalar_tensor_tensor(out=sg.t[:], in0=sgt.t[:], scalar=1.0, in1=b1.t[:],
                                                         op0=ALU.add, op1=ALU.mult), reads=[sgt, b1], writes=[sg])
            return sg

        def stage_b(si, sg):
            hh, tg = steps[si]
            xa = xas[hh]
            sl = slice(tg * 512, (tg + 1) * 512)
            cw = lambda j: sp.t[:, 16 + hh * 4 + j:16 + hh * 4 + j + 1]
            xc = self.wk("lru_xc", [128, 512], F32)
            P.op("dve", lambda h: h.tensor_scalar(
                out=xc.t[:], in0=xa.t[:, tg, 0:512], scalar1=cw(0), scalar2=sp.t[:, 64 + hh:65 + hh],
                op0=ALU.mult, op1=ALU.add), reads=[xa, sp], writes=[xc])
            for j in range(1, 4):
                P.op("dve", lambda h: h.scalar_tensor_tensor(
                    out=xc.t[:], in0=xa.t[:, tg, j:j + 512], scalar=cw(j), in1=xc.t[:],
                    op0=ALU.mult, op1=ALU.add), reads=[xa, sp], writes=[xc])
            xcb = self.wk("lru_xcb", [128, 512], BF16)
            P.op("act", lambda h: h.activation(out=xcb.t[:], in_=xc.t[:], func=AF.Copy), reads=[xc], writes=[xcb])
            br = self.banks[2 + 4 * (si % 2)]
            bi = self.banks[3 + 4 * (si % 2)]
            P.op("pe", lambda h: h.matmul(br.t[:], wg.t[:, hh * 128:(hh + 1) * 128], xcb.t[:],
                                          start=True, stop=True), reads=[wg, xcb], writes=[br])
            P.op("pe", lambda h: h.matmul(bi.t[:], wg.t[:, 1536 + hh * 128:1536 + (hh + 1) * 128], xcb.t[:],
                                          start=True, stop=True), reads=[wg, xcb], writes=[bi])
            rr = self.wk("lru_r", [128, 512], F32)
            ii = self.wk("lru_i", [128, 512], F32)
            P.op("act", lambda h: h.activation(out=rr.t[:], in_=br.t[:], func=AF.Tanh, scale=0.5,
                                               bias=hbias.t[:, hh:hh + 1]), reads=[br, hbias], writes=[rr])
            P.op("act", lambda h: h.activation(out=ii.t[:], in_=bi.t[:], func=AF.Tanh, scale=0.5,
                                               bias=hbias.t[:, 12 + hh:13 + hh]), reads=[bi, hbias], writes=[ii])
            aa = self.wk("lru_a", [128, 512], F32)
            P.op("act", lambda h: h.activation(out=aa.t[:], in_=rr.t[:], func=AF.Exp,
                                               scale=hbias.t[:, 24 + hh:25 + hh], bias=hbias.t[:, 24 + hh:25 + hh]),
                 reads=[rr, hbias], writes=[aa])
            P.op("dve", lambda h: h.tensor_tensor(out=rr.t[:], in0=aa.t[:], in1=aa.t[:], op=ALU.mult),
                 reads=[aa], writes=[rr])
            P.op("act", lambda h: h.activation(out=rr.t[:], in_=rr.t[:], func=AF.Sqrt, scale=-1.0 / 16.0,
                                               bias=six.t[:, 0:1]), reads=[six], writes=[rr])
            P.op("dve", lambda h: h.scalar_tensor_tensor(out=ii.t[:], in0=ii.t[:], scalar=1.0, in1=xc.t[:],
                                                         op0=ALU.add, op1=ALU.mult), reads=[xc], writes=[ii])
            P.op("dve", lambda h: h.tensor_tensor(out=ii.t[:], in0=ii.t[:], in1=rr.t[:], op=ALU.mult),
                 reads=[rr], writes=[ii])
            hb = self.wk("lru_h", [128, 512], F32, n=3)
            hp = hprev.get(hh) if tg > 0 else None
            init = 0.0 if hp is None else hp.t[:, 511:512]
            rds = [aa, ii] + ([hp] if hp is not None else [])
            P.op("dve", lambda h: h.tensor_tensor_scan(
                out=hb.t[:], data0=aa.t[:], data1=ii.t[:], initial=init, op0=ALU.mult, op1=ALU.add),
                reads=rds, writes=[hb])
            hprev[hh] = hb
            P.op("pool", lambda h: h.tensor_tensor(
                out=self.yT_t[:, hh, sl], in0=hb.t[:], in1=sg.t[:], op=ALU.mult),
                reads=[hb, sg], writes=[self.yT[hh]])

        sgs = {0: stage_a(0)}
        for si in range(len(steps)):
            if si + 1 < len(steps):
                sgs[si + 1] = stage_a(si + 1)
            stage_b(si, sgs.pop(si))

    def attention(self, li):
        P = self.P
        d = self.Ld[li]
        scale = 128.0 ** -0.5
        for j in range(4):
            am = self.wk("amask", [128, 3 * 256], F32, n=2)
            P.dma("sp", am.t[:], self.amask_d.t[j, :, :], am, am)
            for g in range(3):
                dil = DIL[g]
                for base, dstc in ((24, g), (36, 3 + g)):
                    ws = self.load_w(d["win"].t[base + g * 4 + j, :, :])
                    for tg in range(4):
                        bank = self.banks[tg % 2]
                        self.proj_fm(ws, tg, bank)
                        if tg % 2 == 0:
                            P.op("act", lambda h: h.activation(
                                out=self.yT_t[:, dstc, tg * 512:(tg + 1) * 512], in_=bank.t[:], func=AF.Copy),
                                reads=[bank], writes=[self.yT[dstc]])
                        else:
                            P.op("dve", lambda h: h.tensor_copy(
                                out=self.yT_t[:, dstc, tg * 512:(tg + 1) * 512], in_=bank.t[:]),
                                reads=[bank], writes=[self.yT[dstc]])
                ws = self.load_w(d["win"].t[48 + g * 4 + j, :, :])
                nb = 16 // dil
                for blk4 in range(4):
                    bank = self.banks[blk4 % 2]
                    for b_ in range(4):
                        blk = blk4 * 4 + b_
                        r, n = blk // nb, blk % nb
                        t0 = r + dil * 128 * n
                        self._tm_first = (b_ == 0)
                        self._tm_last = (b_ == 3)
                        self.proj_tm(ws, bank, slice(b_ * 128, (b_ + 1) * 128), slice(t0, t0 + dil * 127 + 1, dil))
                    P.op("act", lambda h: h.activation(
                        out=self.yT_t[:, 6 + g, blk4 * 512:(blk4 + 1) * 512], in_=bank.t[:], func=AF.Copy),
                        reads=[bank], writes=[self.yT[6 + g]])
            ws = self.load_w(d["win"].t[60 + j, :, :])
            sgb = self.wk("att_sgb", [128, T], BF16, n=1)
            for tg in range(4):
                bank = self.banks[tg % 2]
                self.proj_fm(ws, tg, bank)
                P.op("act", lambda h: h.activation(
                    out=sgb.t[:, tg * 512:(tg + 1) * 512], in_=bank.t[:], func=AF.Silu),
                    reads=[bank], writes=[sgb])
            for R in range(4):
                ub = self.banks[4 + 2 * (R % 2)]
                db = self.banks[5 + 2 * (R % 2)]
                units = []
                for n in range(4 * R, 4 * R + 4):
                    qs = slice(128 * n, 128 * n + 128)
                    kb = []
                    if n >= 1:
                        kb.append((slice(128 * (n - 1), 128 * n), n - 1, 0))
                    kb.append((qs, n, 1))
                    units.append((0, qs, slice((n - 4 * R) * 128, (n - 4 * R) * 128 + 128), 128, kb, 0))
                for r in range(4):
                    q0 = r + 4 * 128 * R
                    qs = slice(q0, q0 + 4 * 127 + 1, 4)
                    kb = []
                    if R >= 1:
                        k0 = r + 4 * 128 * (R - 1)
                        kb.append((slice(k0, k0 + 4 * 127 + 1, 4), r * 4 + R - 1, 0))
                    kb.append((qs, r * 4 + R, 1))
                    units.append((1, qs, slice(r, r + 4 * 127 + 1, 4), 128, kb, 0))
                for r in range(16):
                    q0 = r + 16 * 32 * R
                    qs = slice(q0, q0 + 16 * 31 + 1, 16)
                    kb = [(slice(r, r + 16 * 127 + 1, 16), r, 1)]
                    units.append((2, qs, slice(r, r + 16 * 31 + 1, 16), 32, kb, 32 * R))
                nun = len(units)

                def emit_s(ui):
                    g, qs, osl, nq, kb, moff0 = units[ui]
                    st = self.banks[2 + ui % 2]
                    nkb = len(kb)
                    for b_, (ks, vblk, cur) in enumerate(kb):
                        P.op("pe", lambda h: h.matmul(
                            st.t[:, b_ * nq:(b_ + 1) * nq], self.yT_t[:, 3 + g, ks], self.yT_t[:, g, qs],
                            start=(b_ == 0), stop=(b_ == nkb - 1)),
                            reads=[self.yT[3 + g], self.yT[g]], writes=[st])

                def emit_rest(ui):
                    g, qs, osl, nq, kb, moff0 = units[ui]
                    st = self.banks[2 + ui % 2]
                    nkb = len(kb)
                    nk = nkb * nq
                    ex = self.wk("att_ex", [128, 256], F32, n=3)
                    P.op("act", lambda h: h.activation(
                        out=ex.t[:, 0:nk], in_=st.t[:, 0:nk], func=AF.Exp, scale=scale),
                        reads=[st], writes=[ex])
                    pt = self.wk("att_pt", [128, 256], BF16, n=3)
                    for b_, (ks, vblk, cur) in enumerate(kb):
                        mo = g * 256 + cur * 128 + moff0
                        P.op("dve", lambda h: h.tensor_tensor(
                            out=pt.t[:, b_ * nq:(b_ + 1) * nq], in0=ex.t[:, b_ * nq:(b_ + 1) * nq],
                            in1=am.t[:, mo:mo + nq], op=ALU.mult),
                            reads=[ex, am], writes=[pt])
                    return pt

                def emit_ud(ui, pt):
                    g, qs, osl, nq, kb, moff0 = units[ui]
                    nkb = len(kb)
                    for b_, (ks, vblk, cur) in enumerate(kb):
                        first = (ui == 0 and b_ == 0)
                        lastm = (ui == nun - 1 and b_ == nkb - 1)
                        P.op("pe", lambda h: h.matmul(
                            ub.t[:, osl], self.yT_t[:, 6 + g, vblk * 128:(vblk + 1) * 128],
                            pt.t[:, b_ * nq:(b_ + 1) * nq], start=first, stop=lastm),
                            reads=[self.yT[6 + g], pt], writes=[ub])
                        P.op("pe", lambda h: h.matmul(
                            db.t[:, osl], self.ones.t[:], pt.t[:, b_ * nq:(b_ + 1) * nq],
                            start=first, stop=lastm), reads=[self.ones, pt], writes=[db])

                emit_s(0)
                for ui in range(nun):
                    if ui + 1 < nun:
                        emit_s(ui + 1)
                    pt = emit_rest(ui)
                    emit_ud(ui, pt)
                rec = self.wk("att_rec", [128, 512], F32)
                P.op("dve", lambda h: h.reciprocal(out=rec.t[:], in_=db.t[:]), reads=[db], writes=[rec])
                P.op("dve", lambda h: h.tensor_tensor(out=rec.t[:], in0=ub.t[:], in1=rec.t[:], op=ALU.mult),
                     reads=[ub], writes=[rec])
                P.op("pool", lambda h: h.tensor_tensor(
                    out=self.yT_t[:, 12 + j, R * 512:(R + 1) * 512], in0=rec.t[:], in1=sgb.t[:, R * 512:(R + 1) * 512],
                    op=ALU.mult), reads=[rec, sgb], writes=[self.yT[12 + j]])

    def odd_layer(self, li):
        self.gla(li)
        self.end_phase()
        self.sgu(li)
        self.end_phase()

    def gla(self, li):
        P = self.P
        d = self.Ld[li]
        sp = self.spar
        C8, C9, C10, C11, C13 = 8, 9, 10, 11, 13
        yf = self.yflat
        cst = self.cst
        triT = cst.t[:, 0:128]
        uT = cst.t[:, 128:256]
        glam = cst.t[:, 256:384]
        aT = self.palloc("gla_aT", [17, T], F32)
        wal = self.palloc("gla_wal", [17, 512], F32)
        wina = self.palloc("gla_wina", [128, 256], BF16)
        P.dma("sp", wal.t[:], d["wal"].t[:, :], wal, wal)
        P.dma("pool", wina.t[:], d["wina"].t[:, :], wina, wina)
        P.op("dve", lambda h: h.memset(aT.t[:], 1.0), writes=[aT])
        for tg in range(4):
            bank = self.banks[tg % 2]
            for kc in range(NCH):
                P.op("pe", lambda h: h.matmul(
                    bank.t[0:16, :], wina.t[:, kc * 16:(kc + 1) * 16], self.hT_t[:, kc, tg * 512:(tg + 1) * 512],
                    start=(kc == 0), stop=(kc == NCH - 1)), reads=[wina, self.hT[kc]], writes=[bank])
            P.op("act", lambda h: h.activation(out=aT.t[0:16, tg * 512:(tg + 1) * 512], in_=bank.t[0:16, :],
                                               func=AF.Copy), reads=[bank], writes=[aT])
        S = self.palloc("gla_S", [128, 256], F32)
        Sb = [self.palloc(f"gla_Sb{i}", [128, 256], BF16) for i in range(2)]
        dec = self.palloc("gla_dec", [128, 32], F32)
        for hd in range(4):
            wq = self.load_w(d["win"].t[0 + hd, :, :])
            wk_ = self.load_w(d["win"].t[4 + hd, :, :])
            wv0 = self.load_w(d["win"].t[8 + 2 * hd, :, :])
            wv1 = self.load_w(d["win"].t[9 + 2 * hd, :, :])
            wgc = [self.load_w(d["win"].t[16 + 2 * hd + vc, :, :]) for vc in range(2)]
            P.op("dve", lambda h: h.memset(S.t[:], 0.0), writes=[S])
            P.op("dve", lambda h: h.memset(Sb[0].t[:], 0.0), writes=[Sb[0]])
            for tg in range(4):
                sl = slice(tg * 512, (tg + 1) * 512)
                b2 = self.banks[2]
                for ti in range(4):
                    tk0 = tg * 512 + ti * 128
                    P.op("pe", lambda h: h.matmul(
                        b2.t[:, ti * 128:(ti + 1) * 128], aT.t[0:17, tk0:tk0 + 128], wal.t[0:17, hd * 128:(hd + 1) * 128],
                        start=(ti == 0), stop=(ti == 3)), reads=[aT, wal], writes=[b2])
                L = self.wk("gla_L", [128, 512], F32)
                P.op("act", lambda h: h.activation(out=L.t[:], in_=b2.t[:], func=AF.Exp, scale=-1.0),
                     reads=[b2], writes=[L])
                P.op("act", lambda h: h.activation(out=L.t[:], in_=L.t[:], func=AF.Ln, bias=self.oneb.t[:, 0:1]),
                     reads=[self.oneb], writes=[L])
                b3 = self.banks[3]
                for ti in range(4):
                    P.op("pe", lambda h: h.matmul(
                        b3.t[:, ti * 128:(ti + 1) * 128], L.t[:, ti * 128:(ti + 1) * 128], triT,
                        start=(ti == 0), stop=(ti == 3)), reads=[L, cst], writes=[b3])
                Eb = self.wk("gla_Eb", [128, 512], F32)
                Enb = self.wk("gla_Enb", [128, 512], F32)
                P.op("act", lambda h: h.activation(out=Eb.t[:], in_=b3.t[:], func=AF.Exp), reads=[b3], writes=[Eb])
                P.op("act", lambda h: h.activation(out=Enb.t[:], in_=b3.t[:], func=AF.Exp, scale=-1.0),
                     reads=[b3], writes=[Enb])
                P.op("dve", lambda h: h.tensor_copy(out=dec.t[:, tg * 8:(tg + 1) * 8], in_=Eb.t[:, 63:512:64]),
                     reads=[Eb], writes=[dec])
                for ti in range(4):
                    P.op("pe", lambda h: h.matmul(
                        b2.t[:, ti * 128:(ti + 1) * 128], uT, L.t[:, ti * 128:(ti + 1) * 128],
                        start=(ti == 0), stop=(ti == 3)), reads=[L, cst], writes=[b2])
                Ed = self.wk("gla_Ed", [128, 512], F32)
                P.op("act", lambda h: h.activation(out=Ed.t[:], in_=b2.t[:], func=AF.Exp), reads=[b2], writes=[Ed])
                bq = self.banks[0]
                self.proj_fm(wq, tg, bq)
                P.op("dve", lambda h: h.scalar_tensor_tensor(
                    out=self.yT_t[:, C8, sl], in0=bq.t[:], scalar=128.0 ** -0.5, in1=Eb.t[:],
                    op0=ALU.mult, op1=ALU.mult), reads=[bq, Eb], writes=[self.yT[C8]])
                bk = self.banks[1]
                self.proj_fm(wk_, tg, bk)
                P.op("dve", lambda h: h.tensor_tensor(
                    out=self.yT_t[:, C9, sl], in0=bk.t[:], in1=Enb.t[:], op=ALU.mult),
                    reads=[bk, Enb], writes=[self.yT[C9]])
                bkt = self.banks[0]
                for ti in range(4):
                    tk0 = tg * 512 + ti * 128
                    self._tm_first = (ti == 0)
                    self._tm_last = (ti == 3)
                    self.proj_tm(wk_, bkt, slice(ti * 128, (ti + 1) * 128), slice(tk0, tk0 + 128))
                P.op("dve", lambda h: h.tensor_tensor(
                    out=self.yT_t[:, C10, sl], in0=bkt.t[:], in1=Ed.t[:], op=ALU.mult),
                    reads=[bkt, Ed], writes=[self.yT[C10]])
                for half in range(2):
                    bv = self.banks[1] if half == 0 else self.banks[0]
                    for t2 in range(2):
                        ti = half * 2 + t2
                        tk0 = tg * 512 + ti * 128
                        for vc, wv in enumerate((wv0, wv1)):
                            self._tm_first = (t2 == 0 and vc == 0)
                            self._tm_last = (t2 == 1 and vc == 1)
                            self.proj_tm(wv, bv, slice(t2 * 256 + vc * 128, t2 * 256 + (vc + 1) * 128),
                                         slice(tk0, tk0 + 128))
                    o0 = C11 * T + (tg * 4 + half * 2) * 256
                    P.op("act", lambda h: h.activation(out=yf[:, o0:o0 + 512], in_=bv.t[:], func=AF.Copy),
                         reads=[bv], writes=[self.yT[C11], self.yT[C11 + 1]])
                for vc in range(2):
                    bg = self.banks[vc]
                    self.proj_fm(wgc[vc], tg, bg)
                    P.op("act", lambda h: h.activation(out=self.yT_t[:, C13 + vc, sl], in_=bg.t[:], func=AF.Silu),
                         reads=[bg], writes=[self.yT[C13 + vc]])
                ob = [self.banks[4], self.banks[5]]
                for ti in range(4):
                    tile = tg * 4 + ti
                    tk0 = tile * 128
                    b6 = self.banks[6]
                    P.op("pe", lambda h: h.matmul(b6.t[:, 0:128], self.yT_t[:, C9, tk0:tk0 + 128],
                                                  self.yT_t[:, C8, tk0:tk0 + 128], start=True, stop=True),
                         reads=[self.yT[C8], self.yT[C9]], writes=[b6])
                    attm = self.wk("gla_attm", [128, 128], BF16)
                    P.op("dve", lambda h: h.tensor_tensor(out=attm.t[:], in0=b6.t[:, 0:128], in1=glam, op=ALU.mult),
                         reads=[b6, cst], writes=[attm])
                    for half in range(2):
                        c = tile * 2 + half
                        q0 = c * 64
                        sbc = Sb[c % 2]
                        sbn = Sb[(c + 1) % 2]
                        for vc in range(2):
                            P.op("pe", lambda h: h.matmul(
                                ob[vc].t[:, ti * 128 + half * 64: ti * 128 + half * 64 + 64],
                                sbc.t[:, vc * 128:(vc + 1) * 128], self.yT_t[:, C8, q0:q0 + 64],
                                start=(ti == 0 and half == 0), stop=False),
                                reads=[sbc, self.yT[C8]], writes=[ob[vc]])
                        b7 = self.banks[7]
                        kd0 = C10 * T + tile * 128
                        v0 = C11 * T + tile * 256
                        ps_ = slice(half * 64, (half + 1) * 64)
                        P.op("pe", lambda h: h.matmul(
                            b7.t[:, 0:256], yf[ps_, kd0:kd0 + 128], yf[ps_, v0:v0 + 256], start=True, stop=True),
                            reads=[self.yT[C10], self.yT[C11], self.yT[C11 + 1]], writes=[b7])
                        P.op("dve", lambda h: h.scalar_tensor_tensor(
                            out=S.t[:], in0=S.t[:], scalar=dec.t[:, c:c + 1], in1=b7.t[:, 0:256],
                            op0=ALU.mult, op1=ALU.add), reads=[dec, b7], writes=[S])
                        P.op("act", lambda h: h.activation(out=sbn.t[:], in_=S.t[:], func=AF.Copy),
                             reads=[S], writes=[sbn])
                    for vc in range(2):
                        vv = C11 * T + tile * 256 + vc * 128
                        P.op("pe", lambda h: h.matmul(
                            ob[vc].t[:, ti * 128:(ti + 1) * 128], yf[:, vv:vv + 128], attm.t[:],
                            start=False, stop=(ti == 3)),
                            reads=[self.yT[C11], self.yT[C11 + 1], attm], writes=[ob[vc]])
                b3 = self.banks[3]
                for vc in range(2):
                    sq = self.wk("gla_sq", [128, 512], BF16)
                    P.op("act", lambda h: h.activation(out=sq.t[:], in_=ob[vc].t[:], func=AF.Square),
                         reads=[ob[vc]], writes=[sq])
                    P.op("pe", lambda h: h.matmul(b3.t[:], self.ones.t[:], sq.t[:], start=(vc == 0), stop=(vc == 1)),
                         reads=[self.ones, sq], writes=[b3])
                rs = self.wk("gla_rs", [128, 512], F32)
                P.op("act", lambda h: h.activation(out=rs.t[:], in_=b3.t[:], func=AF.Sqrt, scale=1.0 / 256.0,
                                                   bias=self.epsb.t[:, 0:1]), reads=[b3, self.epsb], writes=[rs])
                P.op("dve", lambda h: h.reciprocal(out=rs.t[:], in_=rs.t[:]), reads=[], writes=[rs])
                for vc in range(2):
                    t1 = self.wk("gla_t1", [128, 512], F32)
                    hn = sp.t[:, 16 + hd * 2 + vc:16 + hd * 2 + vc + 1]
                    P.op("dve", lambda h: h.scalar_tensor_tensor(
                        out=t1.t[:], in0=ob[vc].t[:], scalar=hn, in1=rs.t[:], op0=ALU.mult, op1=ALU.mult),
                        reads=[ob[vc], sp, rs], writes=[t1])
                    P.op("pool", lambda h: h.tensor_tensor(
                        out=self.yT_t[:, 2 * hd + vc, sl], in0=t1.t[:], in1=self.yT_t[:, C13 + vc, sl], op=ALU.mult),
                        reads=[t1, self.yT[C13 + vc]], writes=[self.yT[2 * hd + vc]])

    def sgu(self, li):
        P = self.P
        d = self.Ld[li]
        sp = self.spar
        cst = self.cst
        wsf = self.palloc("sgu_wsf", [128, 512], F32)
        wm = self.palloc("sgu_wm", [128, 512], BF16)
        bsb = self.palloc("sgu_bsb", [128, 512], F32)
        bsr = self.palloc("sgu_bsr", [128, 512], F32)
        P.dma("sp", wsf.t[:], d["wsT"].t[:, :], wsf, wsf)
        P.dma("sp", bsb.t[:], d["bsb"].t[:, :], bsb, bsb)
        for g in range(4):
            P.op("dve", lambda h: h.tensor_tensor(out=wm.t[:, g * 128:(g + 1) * 128], in0=wsf.t[:, g * 128:(g + 1) * 128],
                                                  in1=cst.t[:, 384:512], op=ALU.mult), reads=[wsf, cst], writes=[wm])
        m = self.palloc("sgu_m", [128, 2, T], BF16)
        vn = self.palloc("sgu_vn", [128, 16, 256], BF16)
        for g in range(4):
            for ti in range(4):
                P.op("dve", lambda h: h.tensor_copy(out=bsr.t[:, ti * 128:(ti + 1) * 128],
                                                    in_=bsb.t[:, g * 128:(g + 1) * 128]), reads=[bsb], writes=[bsr])
            for ec in range(2):
                wu = self.load_w(d["win"].t[24 + 2 * g + ec, :, :])
                wgd = self.load_w(d["win"].t[40 + 2 * g + ec, :, :])
                for tg in range(4):
                    sl = slice(tg * 512, (tg + 1) * 512)
                    b0, b1 = self.banks[0], self.banks[1]
                    self.proj_fm(wu, tg, b0)
                    self.proj_fm(wgd, tg, b1)
                    gu = self.wk("sgu_gu", [128, 512], F32)
                    sg = self.wk("sgu_sg", [128, 512], F32)
                    P.op("act", lambda h: h.activation(out=gu.t[:], in_=b0.t[:], func=AF.Gelu_apprx_tanh),
                         reads=[b0], writes=[gu])
                    P.op("act", lambda h: h.activation(out=sg.t[:], in_=b1.t[:], func=AF.Silu),
                         reads=[b1], writes=[sg])
                    P.op("pool", lambda h: h.tensor_tensor(out=m.t[:, ec, sl], in0=gu.t[:], in1=sg.t[:], op=ALU.mult),
                         reads=[gu, sg], writes=[m])
            wv = [self.load_w(d["win"].t[32 + 2 * g + ec, :, :]) for ec in range(2)]
            for tile in range(16):
                bank = self.banks[2 + tile % 2]
                tk0 = tile * 128
                for ec in range(2):
                    self._tm_first = (ec == 0)
                    self._tm_last = (ec == 1)
                    self.proj_tm(wv[ec], bank, slice(ec * 128, (ec + 1) * 128), slice(tk0, tk0 + 128))
                gv = self.wk("sgu_gv", [128, 256], F32)
                P.op("act", lambda h: h.activation(out=gv.t[:], in_=bank.t[:, 0:256], func=AF.Gelu_apprx_tanh),
                     reads=[bank], writes=[gv])
                st6 = self.wk("sgu_st6", [128, 6], F32)
                mv = self.wk("sgu_mv", [128, 2], F32)
                P.op("dve", lambda h: h.bn_stats(out=st6.t[:], in_=gv.t[:]), reads=[gv], writes=[st6])
                P.op("dve", lambda h: h.bn_aggr(out=mv.t[:], in_=st6.t[:]), reads=[st6], writes=[mv])
                sd = self.wk("sgu_sd", [128, 1], F32)
                P.op("act", lambda h: h.activation(out=sd.t[:], in_=mv.t[:, 1:2], func=AF.Sqrt,
                                                   bias=self.epsb.t[:, 0:1]), reads=[mv, self.epsb], writes=[sd])
                P.op("dve", lambda h: h.reciprocal(out=sd.t[:], in_=sd.t[:]), reads=[], writes=[sd])
                P.op("dve", lambda h: h.tensor_scalar(out=vn.t[:, tile, :], in0=gv.t[:], scalar1=mv.t[:, 0:1],
                                                      scalar2=sd.t[:, 0:1], op0=ALU.subtract, op1=ALU.mult),
                     reads=[gv, mv, sd], writes=[vn])
            for ec in range(2):
                for tg in range(4):
                    sl = slice(tg * 512, (tg + 1) * 512)
                    bank = self.banks[4 + (ec * 4 + tg) % 2]
                    for ti in range(4):
                        tile = tg * 4 + ti
                        P.op("pe", lambda h: h.matmul(
                            bank.t[:, ti * 128:(ti + 1) * 128], vn.t[:, tile, ec * 128:(ec + 1) * 128],
                            wm.t[:, g * 128:(g + 1) * 128], start=(ti == 0), stop=(ti == 3)),
                            reads=[vn, wm], writes=[bank])
                    t1 = self.wk("sgu_t1", [128, 512], F32)
                    vnm = sp.t[:, 24 + g * 2 + ec:24 + g * 2 + ec + 1]
                    P.op("dve", lambda h: h.scalar_tensor_tensor(
                        out=t1.t[:], in0=bank.t[:], scalar=vnm, in1=bsr.t[:], op0=ALU.mult, op1=ALU.add),
                        reads=[bank, sp, bsr], writes=[t1])
                    P.op("pool", lambda h: h.tensor_tensor(
                        out=self.yT_t[:, 8 + 2 * g + ec, sl], in0=t1.t[:], in1=m.t[:, ec, sl], op=ALU.mult),
                        reads=[t1, m], writes=[self.yT[8 + 2 * g + ec]])

    def build(self):
        nl = len(self.layers)
        self.load_params(0)
        self.first_norm()
        for li, kind in enumerate(self.layers):
            if kind == "e":
                self.even_layer(li)
            else:
                self.odd_layer(li)
            last = (li == nl - 1)
            if not last:
                pass
            self.out_phase(li, last)
            if not last:
                self.load_params(li + 1)
        return self.nc


def _tile_w(w, ncol_chunks=None):
    K, C = w.shape
    cc = C // 128
    a = w.reshape(K // 128, 128, cc, 128)
    a = np.ascontiguousarray(a.transpose(2, 1, 0, 3))
    return a.reshape(cc, 128, (K // 128) * 128)


def _pp(v):
    return np.ascontiguousarray(v.reshape(-1, 128).T)


def _consts():
    s = np.arange(128)[:, None]
    t = np.arange(128)[None, :]
    same = (s // 64) == (t // 64)
    triT = np.where(same & (s <= t), -1.0 / 16.0, 0.0)
    uT = np.where(same & (s > t), -1.0 / 16.0, 0.0)
    glam = np.where(same & (s <= t), 1.0, 0.0)
    sgm = np.where(s <= t, 1.0, 0.0)
    cst = np.concatenate([triT, uT, glam, sgm], axis=1).astype(np.float32)
    am = np.zeros((4, 128, 3, 256), np.float64)
    k = np.arange(128)[:, None].astype(np.float64)
    q = np.arange(128)[None, :].astype(np.float64)
    for g in range(3):
        for j in range(4):
            slope = 2.0 ** (-8.0 * (g + 3 * j + 1) / 12.0)
            dil = DIL[g]
            prev = np.where(k >= q, np.exp(-slope * dil * (q + 128 - k)), 0.0)
            cur = np.where(k <= q, np.exp(-slope * dil * (q - k)), 0.0)
            am[j, :, g, 0:128] = prev
            am[j, :, g, 128:256] = cur
    return cst, am.reshape(4, 128, 768).astype(np.float32)


LAYERS = ["e", "o", "e", "o"]


def prep_shared(inp, layers):
    f = lambda a: np.ascontiguousarray(np.asarray(a, dtype=np.float32))
    m = {}
    cst, am = _consts()
    m["cst"] = cst
    m["amask"] = am
    m["fg"] = _pp(f(inp["final_norm"]))
    ie = io = 0
    for li, kind in enumerate(layers):
        if kind == "e":
            i = ie
            ie += 1
            m[f"L{li}_win"] = _tile_w(f(inp["ev_w_in"][i]))
            m[f"L{li}_wout"] = _tile_w(f(inp["ev_w_out"][i]))
            sp = np.zeros((128, 112), np.float32)
            sp[:, 0:16] = _pp(f(inp["ev_norm"][i]))
            cw = f(inp["ev_conv_w"][i])
            sp[:, 16:64] = cw.reshape(4, 12, 128).transpose(2, 1, 0).reshape(128, 48)
            sp[:, 64:76] = _pp(f(inp["ev_conv_b"][i]))
            sp[:, 76:88] = _pp(f(inp["ev_lam"][i]))
            sp[:, 88:100] = _pp(f(inp["ev_b_a"][i]).reshape(-1))
            sp[:, 100:112] = _pp(f(inp["ev_b_i"][i]).reshape(-1))
            m[f"L{li}_sp"] = sp
            wa = f(inp["ev_w_a"][i]).transpose(1, 0, 2).reshape(128, 1536)
            wi = f(inp["ev_w_i"][i]).transpose(1, 0, 2).reshape(128, 1536)
            m[f"L{li}_wg"] = np.ascontiguousarray(np.concatenate([wa, wi], axis=1))
        else:
            i = io
            io += 1
            w = f(inp["od_w_in"][i])
            cols = np.concatenate([np.arange(0, 2048), np.arange(2064, 6160)])
            m[f"L{li}_win"] = _tile_w(np.ascontiguousarray(w[:, cols]))
            wa = w[:, 2048:2064].reshape(16, 128, 16).transpose(1, 0, 2).reshape(128, 256)
            m[f"L{li}_wina"] = np.ascontiguousarray(wa)
            m[f"L{li}_wout"] = _tile_w(f(inp["od_w_out"][i]))
            sp = np.zeros((128, 32), np.float32)
            sp[:, 0:16] = _pp(f(inp["od_norm"][i]))
            sp[:, 16:24] = _pp(f(inp["od_head_norm"][i]).reshape(-1))
            sp[:, 24:32] = _pp(f(inp["od_v_norm"][i]).reshape(-1))
            m[f"L{li}_sp"] = sp
            m[f"L{li}_wal"] = np.ascontiguousarray(
                np.concatenate([f(inp["od_w_alpha"][i]), f(inp["od_b_alpha"][i])[None, :]], axis=0))
            ws = f(inp["od_w_s"][i])
            m[f"L{li}_wsT"] = np.ascontiguousarray(ws.transpose(2, 0, 1).reshape(128, 512))
            bs = f(inp["od_b_s"][i]).reshape(1, 512)
            m[f"L{li}_bsb"] = np.ascontiguousarray(np.broadcast_to(bs, (128, 512)))
    return m


def run_layers(x, inp, layers, final=True):
    b = Builder(layers, final=final)
    nc = b.build()
    shared = prep_shared(inp, layers)
    in_maps = []
    for core in range(N_CORES):
        bi = core % 4
        xt = np.ascontiguousarray(x[bi].T).reshape(NCH, 128, T)
        mm = dict(shared)
        mm["xin"] = xt
        in_maps.append(mm)
    res = run_bass_kernel_spmd(nc, in_maps, core_ids=list(range(N_CORES)))
    out = np.empty((4, T, D), np.float32)
    for bi in range(4):
        out[bi] = res.results[bi]["yout"].reshape(D, T).T
    return out


def kernel(**inputs):
    x = np.asarray(inputs["x"], dtype=np.float32)
    return run_layers(x, inputs, LAYERS, final=True)
```

```python
import numpy as np
import concourse.bass as bass
import concourse.mybir as mybir
from concourse.bass_utils import run_bass_kernel_spmd

F32 = mybir.dt.float32
BF16 = mybir.dt.bfloat16
AF = mybir.ActivationFunctionType
ALU = mybir.AluOpType

D = 2048
T = 2048
NCH = 16
EPS = 1e-6
SEM_LIMIT = 16000
SAME_ENG_SYNC = True
N_CORES = 8

DIL = (1, 4, 16)


class Tk:
    __slots__ = ("name", "w", "r", "dsem", "dcnt", "t")

    def __init__(self, name, t=None):
        self.name = name
        self.w = None
        self.r = {}
        self.dsem = None
        self.dcnt = 0
        self.t = t


class Eng:
    def __init__(self, name, h):
        self.name = name
        self.h = h
        self.sem = None
        self.cnt = 0
        self.known = {}
        self.nsem = 0


class Prog:
    def __init__(self):
        self.nc = bass.Bass("TRN2", target_bir_lowering=False)
        nc = self.nc
        self.E = {
            "pe": Eng("pe", nc.tensor),
            "act": Eng("act", nc.scalar),
            "dve": Eng("dve", nc.vector),
            "pool": Eng("pool", nc.gpsimd),
            "sp": Eng("sp", nc.sync),
        }
        self.n_inst = 0
        self.n_wait = 0
        self.dma_tks = {}
        self.allsems = []
        self.sem_pool = []
        self.swdge_sems = set()

    def sb(self, name, shape, dt):
        t = self.nc.alloc_sbuf_tensor(name, list(shape), dt)
        return Tk(name, t)

    def ps(self, name):
        t = self.nc.alloc_psum_tensor(name, [128, 512], F32)
        return Tk(name, t)

    def dram(self, name, shape, dt, kind):
        t = self.nc.dram_tensor(name, list(shape), dt, kind=kind)
        return Tk(name, t.ap())

    def _wait(self, e, evs):
        for (sem, val) in evs:
            k = id(sem)
            if e.known.get(k, 0) >= val:
                continue
            e.h.wait_ge(sem, val)
            e.known[k] = val
            self.n_wait += 1

    def _deps(self, e, reads, writes):
        evs = {}

        def add(ev):
            if ev is None:
                return
            k = id(ev[0])
            if k not in evs or evs[k][1] < ev[1]:
                evs[k] = ev

        for t in reads:
            add(t.w)
        for t in writes:
            add(t.w)
            for ev in t.r.values():
                add(ev)
        out = []
        for ev in evs.values():
            if ev[0] is e.sem:
                if e.name == "pe" or (e.name != "pool" and not SAME_ENG_SYNC):
                    continue
            out.append(ev)
        return out

    def _newsem(self, e):
        if e.sem is None or e.cnt >= SEM_LIMIT:
            e.sem = self.nc.alloc_semaphore(f"s_{e.name}_{e.nsem}")
            self.allsems.append(e.sem)
            e.nsem += 1
            e.cnt = 0

    def op(self, en, fn, reads=(), writes=()):
        e = self.E[en]
        self._newsem(e)
        self._wait(e, self._deps(e, reads, writes))
        inst = fn(e.h)
        e.cnt += 1
        inst.then_inc(e.sem, 1)
        ev = (e.sem, e.cnt)
        for t in reads:
            t.r[id(e.sem)] = ev
        for t in writes:
            t.w = ev
            t.r = {}
        self.n_inst += 1
        return inst

    def dma(self, qn, out, in_, dst, semtk, reads=(), **kw):
        e = self.E[qn]
        if semtk.dsem is None:
            if self.sem_pool and qn != "pool":
                semtk.dsem, semtk.dcnt = self.sem_pool.pop()
            else:
                semtk.dsem = self.nc.alloc_semaphore(f"d{len(self.allsems)}_{semtk.name}")
                self.allsems.append(semtk.dsem)
        if qn == "pool":
            self.swdge_sems.add(id(semtk.dsem))
        self.dma_tks[id(semtk)] = semtk
        self._wait(e, self._deps(e, reads, (dst,)))
        inst = e.h.dma_start(out=out, in_=in_, **kw)
        semtk.dcnt += 16
        inst.then_inc(semtk.dsem, 16)
        ev = (semtk.dsem, semtk.dcnt)
        for t in reads:
            t.r[id(semtk.dsem)] = ev
        dst.w = ev
        dst.r = {}
        self.n_inst += 1
        return inst

    def wait_all(self, en, tks):
        e = self.E[en]
        evs = []
        for t in tks:
            if t.w is not None:
                evs.append(t.w)
        self._wait(e, evs)

    def barrier(self):
        evs = []
        for f in self.E.values():
            if f.sem is not None and f.cnt > 0:
                evs.append((f.sem, f.cnt))
        for t in self.dma_tks.values():
            if t.dcnt > 0:
                evs.append((t.dsem, t.dcnt))
        for e in self.E.values():
            self._wait(e, evs)


class Builder:
    def __init__(self, layers, final=True):
        self.P = Prog()
        P = self.P
        self.layers = layers
        self.n_lru = 12
        self.final = final
        nc = P.nc
        self.nc = nc
        self.xin = P.dram("xin", [NCH, 128, T], F32, "ExternalInput")
        self.yout = P.dram("yout", [NCH, 128, T], F32, "ExternalOutput")
        self.youts = [[Tk(f"yo{c}_{g}") for g in range(4)] for c in range(NCH)]
        self.xs_ap = nc.dram_tensor("xs", [NCH, 128, T], F32, kind="Internal").ap()
        self.xs = [[Tk(f"xs{c}_{g}") for g in range(4)] for c in range(NCH)]
        self.cst_d = P.dram("cst", [128, 4 * 128], F32, "ExternalInput")
        self.amask_d = P.dram("amask", [4, 128, 3 * 256], F32, "ExternalInput")
        self.fg_d = P.dram("fg", [128, NCH], F32, "ExternalInput")
        self.Ld = []
        for li, kind in enumerate(layers):
            d = {}
            if kind == "e":
                d["win"] = P.dram(f"L{li}_win", [64, 128, 2048], F32, "ExternalInput")
                d["wout"] = P.dram(f"L{li}_wout", [16, 128, 2048], F32, "ExternalInput")
                d["sp"] = P.dram(f"L{li}_sp", [128, 112], F32, "ExternalInput")
                d["wg"] = P.dram(f"L{li}_wg", [128, 2 * 12 * 128], F32, "ExternalInput")
            else:
                d["win"] = P.dram(f"L{li}_win", [48, 128, 2048], F32, "ExternalInput")
                d["wina"] = P.dram(f"L{li}_wina", [128, 256], F32, "ExternalInput")
                d["wout"] = P.dram(f"L{li}_wout", [16, 128, 2048], F32, "ExternalInput")
                d["sp"] = P.dram(f"L{li}_sp", [128, 32], F32, "ExternalInput")
                d["wal"] = P.dram(f"L{li}_wal", [17, 512], F32, "ExternalInput")
                d["wsT"] = P.dram(f"L{li}_wsT", [128, 512], F32, "ExternalInput")
                d["bsb"] = P.dram(f"L{li}_bsb", [128, 512], F32, "ExternalInput")
            self.Ld.append(d)

        self.hT_t = nc.alloc_sbuf_tensor("hT", [128, NCH, T], BF16)
        self.hT = [Tk(f"hT{c}", self.hT_t) for c in range(NCH)]
        self.yT_t = nc.alloc_sbuf_tensor("yT", [128, NCH, T], BF16)
        self.yT = [Tk(f"yT{c}", self.yT_t) for c in range(NCH)]
        self.yflat = self.yT_t[:, :, :].rearrange("p c t -> p (c t)")
        self.NSLOT = 6
        self.wslot = [P.sb(f"wslot{i}", [128, NCH * 128], BF16) for i in range(self.NSLOT)]
        self.wnext = 0
        self.ones = P.sb("ones", [128, 128], BF16)
        self.cst = P.sb("cstsb", [128, 512], F32)
        self.spar = P.sb("spar", [128, 112], F32)
        self.sparn = P.sb("sparn", [128, 16], F32)
        self.fg = P.sb("fgsb", [128, NCH], F32)
        self.epsb = P.sb("epsb", [128, 1], F32)
        self.oneb = P.sb("oneb", [128, 1], F32)
        self.banks = [P.ps(f"bank{i}") for i in range(8)]
        self._wk = {}
        self._guards = []
        self._ptks = []
        self._pid = 0

        P.op("dve", lambda h: h.memset(self.ones.t[:], 1.0), writes=[self.ones])
        P.op("dve", lambda h: h.memset(self.epsb.t[:], EPS), writes=[self.epsb])
        P.op("dve", lambda h: h.memset(self.oneb.t[:], 1.0), writes=[self.oneb])
        P.dma("sp", self.cst.t[:], self.cst_d.t[:, :], self.cst, self.cst)
        P.dma("sp", self.fg.t[:], self.fg_d.t[:, :], self.fg, self.fg)

    def palloc(self, name, shape, dt):
        g = self.nc.sbuf_tensor(f"{name}_p{self._pid}", list(shape), dt)
        t = g.__enter__()
        self._guards.append(g)
        tk = Tk(name, t)
        self._ptks.append(tk)
        return tk

    def end_phase(self):
        self.P.barrier()
        for g in reversed(self._guards):
            g.__exit__(None, None, None)
        self._guards = []
        for tk in self._ptks:
            if tk.dsem is not None:
                if id(tk.dsem) not in self.P.swdge_sems:
                    self.P.sem_pool.append((tk.dsem, tk.dcnt))
                self.P.dma_tks.pop(id(tk), None)
                tk.dsem = None
        self._ptks = []
        self._wk = {}
        self._pid += 1

    def wk(self, name, shape, dt, n=2):
        if name not in self._wk:
            self._wk[name] = [[self.palloc(f"{name}_{i}", shape, dt) for i in range(n)], 0]
        lst = self._wk[name]
        t = lst[0][lst[1] % n]
        lst[1] += 1
        return t

    def load_w(self, src_ap):
        P = self.P
        s = self.wslot[self.wnext % self.NSLOT]
        self.wnext += 1
        P.dma("pool", s.t[:, :], src_ap, s, s)
        return s

    def proj_fm(self, ws, tg, bank, nrow=128):
        P = self.P
        for kc in range(NCH):
            P.op("pe", lambda h: h.matmul(
                bank.t[0:nrow, :], ws.t[:, kc * nrow:(kc + 1) * nrow], self.hT_t[:, kc, tg * 512:(tg + 1) * 512],
                start=(kc == 0), stop=(kc == NCH - 1)),
                reads=[ws, self.hT[kc]], writes=[bank])

    def proj_tm(self, ws, bank, osl, tsl):
        P = self.P
        for kc in range(NCH):
            P.op("pe", lambda h: h.matmul(
                bank.t[:, osl], self.hT_t[:, kc, tsl], ws.t[:, kc * 128:(kc + 1) * 128],
                start=(kc == 0 and self._tm_first), stop=(kc == NCH - 1 and self._tm_last)),
                reads=[ws, self.hT[kc]], writes=[bank])

    def norm_stats_step(self, xt, c, tg, ssq, gain_tk, write_h, defer=False):
        P = self.P
        sl = slice(tg * 512, (tg + 1) * 512)
        sq = self.wk("sq", [128, 512], BF16, n=3)
        P.op("act", lambda h: h.activation(out=sq.t[:], in_=xt.t[:], func=AF.Square), reads=[xt], writes=[sq])

        def emit_ssq():
            P.op("pe", lambda h: h.matmul(ssq[tg].t[:], self.ones.t[:], sq.t[:], start=(c == 0), stop=(c == NCH - 1)),
                 reads=[self.ones, sq], writes=[ssq[tg]])

        if write_h:
            P.op("act", lambda h: h.activation(
                out=self.hT_t[:, c, sl], in_=xt.t[:], func=AF.Copy, scale=gain_tk.t[:, c:c + 1]),
                reads=[xt, gain_tk], writes=[self.hT[c]])
        if defer:
            return emit_ssq
        emit_ssq()
        return None

    def first_norm(self):
        P = self.P
        ssq = self.banks[4:8]
        for c in range(NCH):
            for tg in range(4):
                sl = slice(tg * 512, (tg + 1) * 512)
                xt = self.wk("xt", [128, 512], F32, n=3)
                P.dma("sp", xt.t[:], self.xin.t[c, :, sl], xt, xt)
                self.norm_stats_step(xt, c, tg, ssq, self.spar, True)
        self.finish_norm(ssq)
        self.scale_h()
        self.end_phase()

    def finish_norm(self, ssq):
        P = self.P
        self.rstd = self.palloc("rstd", [128, T], F32)
        for tg in range(4):
            sl = slice(tg * 512, (tg + 1) * 512)
            tmp = self.wk("nrm_tmp", [128, 512], F32)
            P.op("act", lambda h: h.activation(out=tmp.t[:], in_=ssq[tg].t[:], func=AF.Sqrt,
                                               scale=1.0 / D, bias=self.epsb.t[:, 0:1]),
                 reads=[ssq[tg], self.epsb], writes=[tmp])
            P.op("dve", lambda h: h.reciprocal(out=self.rstd.t[:, sl], in_=tmp.t[:]),
                 reads=[tmp], writes=[self.rstd])

    def scale_h(self):
        P = self.P
        for c in range(NCH):
            P.op("dve", lambda h: h.tensor_tensor(out=self.hT_t[:, c, :], in0=self.hT_t[:, c, :],
                                               in1=self.rstd.t[:], op=ALU.mult),
                 reads=[self.rstd], writes=[self.hT[c]])

    def out_phase(self, li, last):
        P = self.P
        d = self.Ld[li]
        ssq = self.banks[4:8]
        if not last:
            P.dma("sp", self.sparn.t[:, 0:16], self.Ld[li + 1]["sp"].t[:, 0:16], self.sparn, self.sparn)
        pending = None
        for dc in range(NCH):
            ws = self.load_w(d["wout"].t[dc, :, :])
            for tg in range(4):
                sl = slice(tg * 512, (tg + 1) * 512)
                xt = self.wk("xt", [128, 512], F32, n=4)
                if li == 0:
                    P.dma("sp", xt.t[:], self.xin.t[dc, :, sl], xt, xt)
                else:
                    P.dma("sp", xt.t[:], self.xs_ap[dc, :, sl], xt, xt, reads=[self.xs[dc][tg]])
                bank = self.banks[(dc * 4 + tg) % 2]
                for fc in range(NCH):
                    P.op("pe", lambda h: h.matmul(
                        bank.t[:], ws.t[:, fc * 128:(fc + 1) * 128], self.yT_t[:, fc, sl],
                        start=(fc == 0), stop=(fc == NCH - 1)),
                        reads=[ws, self.yT[fc]], writes=[bank])
                if pending is not None:
                    pending()
                P.op("dve", lambda h: h.tensor_tensor(out=xt.t[:], in0=bank.t[:], in1=xt.t[:], op=ALU.add),
                     reads=[bank], writes=[xt])
                pending = self.norm_stats_step(xt, dc, tg, ssq, self.sparn, not last, defer=True)
                P.dma("sp", self.xs_ap[dc, :, sl], xt.t[:], self.xs[dc][tg], xt, reads=[xt])
        if pending is not None:
            pending()
        self.finish_norm(ssq)
        if not last:
            self.scale_h()
        else:
            self.final_out()
        self.end_phase()

    def final_out(self):
        P = self.P
        for c in range(NCH):
            xf = self.wk("xf", [128, T], F32, n=2)
            P.dma("sp", xf.t[:], self.xs_ap[c, :, :], xf, xf, reads=[self.xs[c][tg] for tg in range(4)])
            if self.final:
                P.op("dve", lambda h: h.scalar_tensor_tensor(
                    out=xf.t[:], in0=xf.t[:], scalar=self.fg.t[:, c:c + 1], in1=self.rstd.t[:],
                    op0=ALU.mult, op1=ALU.mult),
                    reads=[self.fg, self.rstd], writes=[xf])
            P.dma("sp", self.yout.t[c, :, :], xf.t[:], self.youts[c][0], xf, reads=[xf])
        P.wait_all("sp", [self.youts[c][0] for c in range(NCH)])

    def load_params(self, li):
        P = self.P
        d = self.Ld[li]
        n = 112 if self.layers[li] == "e" else 32
        P.dma("sp", self.spar.t[:, 0:n], d["sp"].t[:, :], self.spar, self.spar)

    def even_layer(self, li):
        self.attention(li)
        self.end_phase()
        self.lru(li)
        self.end_phase()

    def lru(self, li):
        P = self.P
        d = self.Ld[li]
        sp = self.spar
        wg = self.palloc("wg", [128, 2 * 12 * 128], BF16)
        for q in range(2):
            P.dma("pool", wg.t[:, q * 1536:(q + 1) * 1536], d["wg"].t[:, q * 1536:(q + 1) * 1536], wg, wg)
        cc = self.palloc("lru_c", [128, 12], F32)
        ee = self.palloc("lru_e", [128, 12], F32)
        t1 = self.palloc("lru_t1", [128, 12], F32)
        P.op("act", lambda h: h.activation(out=ee.t[:], in_=sp.t[:, 76:88], func=AF.Exp, scale=-1.0),
             reads=[sp], writes=[ee])
        P.op("dve", lambda h: h.tensor_scalar(out=t1.t[:], in0=ee.t[:], scalar1=-1.0 / 3.0, scalar2=0.5,
                                              op0=ALU.mult, op1=ALU.add), reads=[ee], writes=[t1])
        P.op("dve", lambda h: h.tensor_tensor(out=t1.t[:], in0=t1.t[:], in1=ee.t[:], op=ALU.mult),
             reads=[ee], writes=[t1])
        P.op("dve", lambda h: h.tensor_scalar(out=t1.t[:], in0=t1.t[:], scalar1=-1.0, scalar2=1.0,
                                              op0=ALU.mult, op1=ALU.add), reads=[], writes=[t1])
        P.op("dve", lambda h: h.tensor_tensor(out=t1.t[:], in0=t1.t[:], in1=ee.t[:], op=ALU.mult),
             reads=[ee], writes=[t1])
        P.op("dve", lambda h: h.tensor_scalar(out=cc.t[:], in0=t1.t[:], scalar1=-8.0, scalar2=None, op0=ALU.mult),
             reads=[t1], writes=[cc])
        NH = self.n_lru
        steps = [(hh, tg) for hh in range(NH) for tg in range(4)]
        wts = {}
        xas = {}
        hprev = {}

        def stage_a(si):
            hh, tg = steps[si]
            if tg == 0:
                wts[hh] = (self.load_w(d["win"].t[hh, :, :]), self.load_w(d["win"].t[NH + hh, :, :]))
                xas[hh] = self.wk("lru_xa", [128, 4, 3 + 512], F32, n=2)
            w_xa, w_ga = wts[hh]
            xa = xas[hh]
            b0 = self.banks[0 + si % 2]
            b1 = self.banks[4 + si % 2]
            self.proj_fm(w_xa, tg, b0)
            self.proj_fm(w_ga, tg, b1)
            P.op("act", lambda h: h.activation(out=xa.t[:, tg, 3:515], in_=b0.t[:], func=AF.Copy),
                 reads=[b0], writes=[xa])
            if tg == 0:
                P.op("dve", lambda h: h.memset(xa.t[:, 0, 0:3], 0.0), writes=[xa])
            else:
                P.op("dve", lambda h: h.tensor_copy(out=xa.t[:, tg, 0:3], in_=xa.t[:, tg - 1, 512:515]),
                     writes=[xa])
            sg = self.wk("lru_sg", [128, 512], BF16, n=3)
            P.op("act", lambda h: h.activation(out=sg.t[:], in_=b1.t[:], func=AF.Silu), reads=[b1], writes=[sg])
            return sg

        def stage_b(si, sg):
            hh, tg = steps[si]
            xa = xas[hh]
            sl = slice(tg * 512, (tg + 1) * 512)
            cw = lambda j: sp.t[:, 16 + hh * 4 + j:16 + hh * 4 + j + 1]
            xc = self.wk("lru_xc", [128, 512], F32)
            P.op("dve", lambda h: h.tensor_scalar(
                out=xc.t[:], in0=xa.t[:, tg, 0:512], scalar1=cw(0), scalar2=sp.t[:, 64 + hh:65 + hh],
                op0=ALU.mult, op1=ALU.add), reads=[xa, sp], writes=[xc])
            for j in range(1, 4):
                P.op("dve", lambda h: h.scalar_tensor_tensor(
                    out=xc.t[:], in0=xa.t[:, tg, j:j + 512], scalar=cw(j), in1=xc.t[:],
                    op0=ALU.mult, op1=ALU.add), reads=[xa, sp], writes=[xc])
            xcb = self.wk("lru_xcb", [128, 512], BF16)
            P.op("act", lambda h: h.activation(out=xcb.t[:], in_=xc.t[:], func=AF.Copy), reads=[xc], writes=[xcb])
            br = self.banks[2 + 4 * (si % 2)]
            bi = self.banks[3 + 4 * (si % 2)]
            P.op("pe", lambda h: h.matmul(br.t[:], wg.t[:, hh * 128:(hh + 1) * 128], xcb.t[:],
                                          start=True, stop=True), reads=[wg, xcb], writes=[br])
            P.op("pe", lambda h: h.matmul(bi.t[:], wg.t[:, 1536 + hh * 128:1536 + (hh + 1) * 128], xcb.t[:],
                                          start=True, stop=True), reads=[wg, xcb], writes=[bi])
            rr = self.wk("lru_r", [128, 512], F32)
            ii = self.wk("lru_i", [128, 512], F32)
            P.op("act", lambda h: h.activation(out=rr.t[:], in_=br.t[:], func=AF.Sigmoid,
                                               bias=sp.t[:, 88 + hh:89 + hh]), reads=[br, sp], writes=[rr])
            P.op("act", lambda h: h.activation(out=ii.t[:], in_=bi.t[:], func=AF.Sigmoid,
                                               bias=sp.t[:, 100 + hh:101 + hh]), reads=[bi, sp], writes=[ii])
            aa = self.wk("lru_a", [128, 512], F32)
            P.op("act", lambda h: h.activation(out=aa.t[:], in_=rr.t[:], func=AF.Exp,
                                               scale=cc.t[:, hh:hh + 1]), reads=[rr, cc], writes=[aa])
            P.op("dve", lambda h: h.tensor_tensor(out=rr.t[:], in0=aa.t[:], in1=aa.t[:], op=ALU.mult),
                 reads=[aa], writes=[rr])
            P.op("act", lambda h: h.activation(out=rr.t[:], in_=rr.t[:], func=AF.Sqrt, scale=-1.0,
                                               bias=self.oneb.t[:, 0:1]), reads=[self.oneb], writes=[rr])
            P.op("dve", lambda h: h.tensor_tensor(out=ii.t[:], in0=ii.t[:], in1=xc.t[:], op=ALU.mult),
                 reads=[xc], writes=[ii])
            P.op("dve", lambda h: h.tensor_tensor(out=ii.t[:], in0=ii.t[:], in1=rr.t[:], op=ALU.mult),
                 reads=[rr], writes=[ii])
            hb = self.wk("lru_h", [128, 512], F32, n=3)
            hp = hprev.get(hh) if tg > 0 else None
            init = 0.0 if hp is None else hp.t[:, 511:512]
            rds = [aa, ii] + ([hp] if hp is not None else [])
            P.op("dve", lambda h: h.tensor_tensor_scan(
                out=hb.t[:], data0=aa.t[:], data1=ii.t[:], initial=init, op0=ALU.mult, op1=ALU.add),
                reads=rds, writes=[hb])
            hprev[hh] = hb
            P.op("pool", lambda h: h.tensor_tensor(
                out=self.yT_t[:, hh, sl], in0=hb.t[:], in1=sg.t[:], op=ALU.mult),
                reads=[hb, sg], writes=[self.yT[hh]])

        sgs = {0: stage_a(0)}
        for si in range(len(steps)):
            if si + 1 < len(steps):
                sgs[si + 1] = stage_a(si + 1)
            stage_b(si, sgs.pop(si))

    def attention(self, li):
        P = self.P
        d = self.Ld[li]
        scale = 128.0 ** -0.5
        for j in range(4):
            am = self.wk("amask", [128, 3 * 256], F32, n=2)
            P.dma("sp", am.t[:], self.amask_d.t[j, :, :], am, am)
            for g in range(3):
                dil = DIL[g]
                for base, dstc in ((24, g), (36, 3 + g)):
                    ws = self.load_w(d["win"].t[base + g * 4 + j, :, :])
                    for tg in range(4):
                        bank = self.banks[tg % 2]
                        self.proj_fm(ws, tg, bank)
                        if tg % 2 == 0:
                            P.op("act", lambda h: h.activation(
                                out=self.yT_t[:, dstc, tg * 512:(tg + 1) * 512], in_=bank.t[:], func=AF.Copy),
                                reads=[bank], writes=[self.yT[dstc]])
                        else:
                            P.op("dve", lambda h: h.tensor_copy(
                                out=self.yT_t[:, dstc, tg * 512:(tg + 1) * 512], in_=bank.t[:]),
                                reads=[bank], writes=[self.yT[dstc]])
                ws = self.load_w(d["win"].t[48 + g * 4 + j, :, :])
                nb = 16 // dil
                for blk4 in range(4):
                    bank = self.banks[blk4 % 2]
                    for b_ in range(4):
                        blk = blk4 * 4 + b_
                        r, n = blk // nb, blk % nb
                        t0 = r + dil * 128 * n
                        self._tm_first = (b_ == 0)
                        self._tm_last = (b_ == 3)
                        self.proj_tm(ws, bank, slice(b_ * 128, (b_ + 1) * 128), slice(t0, t0 + dil * 127 + 1, dil))
                    P.op("act", lambda h: h.activation(
                        out=self.yT_t[:, 6 + g, blk4 * 512:(blk4 + 1) * 512], in_=bank.t[:], func=AF.Copy),
                        reads=[bank], writes=[self.yT[6 + g]])
            ws = self.load_w(d["win"].t[60 + j, :, :])
            sgb = self.wk("att_sgb", [128, T], BF16, n=1)
            for tg in range(4):
                bank = self.banks[tg % 2]
                self.proj_fm(ws, tg, bank)
                P.op("act", lambda h: h.activation(
                    out=sgb.t[:, tg * 512:(tg + 1) * 512], in_=bank.t[:], func=AF.Silu),
                    reads=[bank], writes=[sgb])
            for R in range(4):
                ub = self.banks[4 + 2 * (R % 2)]
                db = self.banks[5 + 2 * (R % 2)]
                units = []
                for n in range(4 * R, 4 * R + 4):
                    qs = slice(128 * n, 128 * n + 128)
                    kb = []
                    if n >= 1:
                        kb.append((slice(128 * (n - 1), 128 * n), n - 1, 0))
                    kb.append((qs, n, 1))
                    units.append((0, qs, slice((n - 4 * R) * 128, (n - 4 * R) * 128 + 128), 128, kb, 0))
                for r in range(4):
                    q0 = r + 4 * 128 * R
                    qs = slice(q0, q0 + 4 * 127 + 1, 4)
                    kb = []
                    if R >= 1:
                        k0 = r + 4 * 128 * (R - 1)
                        kb.append((slice(k0, k0 + 4 * 127 + 1, 4), r * 4 + R - 1, 0))
                    kb.append((qs, r * 4 + R, 1))
                    units.append((1, qs, slice(r, r + 4 * 127 + 1, 4), 128, kb, 0))
                for r in range(16):
                    q0 = r + 16 * 32 * R
                    qs = slice(q0, q0 + 16 * 31 + 1, 16)
                    kb = [(slice(r, r + 16 * 127 + 1, 16), r, 1)]
                    units.append((2, qs, slice(r, r + 16 * 31 + 1, 16), 32, kb, 32 * R))
                nun = len(units)

                def emit_s(ui):
                    g, qs, osl, nq, kb, moff0 = units[ui]
                    st = self.banks[2 + ui % 2]
                    nkb = len(kb)
                    for b_, (ks, vblk, cur) in enumerate(kb):
                        P.op("pe", lambda h: h.matmul(
                            st.t[:, b_ * nq:(b_ + 1) * nq], self.yT_t[:, 3 + g, ks], self.yT_t[:, g, qs],
                            start=(b_ == 0), stop=(b_ == nkb - 1)),
                            reads=[self.yT[3 + g], self.yT[g]], writes=[st])

                def emit_rest(ui):
                    g, qs, osl, nq, kb, moff0 = units[ui]
                    st = self.banks[2 + ui % 2]
                    nkb = len(kb)
                    nk = nkb * nq
                    ex = self.wk("att_ex", [128, 256], F32, n=3)
                    P.op("act", lambda h: h.activation(
                        out=ex.t[:, 0:nk], in_=st.t[:, 0:nk], func=AF.Exp, scale=scale),
                        reads=[st], writes=[ex])
                    pt = self.wk("att_pt", [128, 256], BF16, n=3)
                    for b_, (ks, vblk, cur) in enumerate(kb):
                        mo = g * 256 + cur * 128 + moff0
                        P.op("dve", lambda h: h.tensor_tensor(
                            out=pt.t[:, b_ * nq:(b_ + 1) * nq], in0=ex.t[:, b_ * nq:(b_ + 1) * nq],
                            in1=am.t[:, mo:mo + nq], op=ALU.mult),
                            reads=[ex, am], writes=[pt])
                    return pt

                def emit_ud(ui, pt):
                    g, qs, osl, nq, kb, moff0 = units[ui]
                    nkb = len(kb)
                    for b_, (ks, vblk, cur) in enumerate(kb):
                        first = (ui == 0 and b_ == 0)
                        lastm = (ui == nun - 1 and b_ == nkb - 1)
                        P.op("pe", lambda h: h.matmul(
                            ub.t[:, osl], self.yT_t[:, 6 + g, vblk * 128:(vblk + 1) * 128],
                            pt.t[:, b_ * nq:(b_ + 1) * nq], start=first, stop=lastm),
                            reads=[self.yT[6 + g], pt], writes=[ub])
                        P.op("pe", lambda h: h.matmul(
                            db.t[:, osl], self.ones.t[:], pt.t[:, b_ * nq:(b_ + 1) * nq],
                            start=first, stop=lastm), reads=[self.ones, pt], writes=[db])

                emit_s(0)
                for ui in range(nun):
                    if ui + 1 < nun:
                        emit_s(ui + 1)
                    pt = emit_rest(ui)
                    emit_ud(ui, pt)
                rec = self.wk("att_rec", [128, 512], F32)
                P.op("dve", lambda h: h.reciprocal(out=rec.t[:], in_=db.t[:]), reads=[db], writes=[rec])
                P.op("dve", lambda h: h.tensor_tensor(out=rec.t[:], in0=ub.t[:], in1=rec.t[:], op=ALU.mult),
                     reads=[ub], writes=[rec])
                P.op("pool", lambda h: h.tensor_tensor(
                    out=self.yT_t[:, 12 + j, R * 512:(R + 1) * 512], in0=rec.t[:], in1=sgb.t[:, R * 512:(R + 1) * 512],
                    op=ALU.mult), reads=[rec, sgb], writes=[self.yT[12 + j]])

    def odd_layer(self, li):
        self.gla(li)
        self.end_phase()
        self.sgu(li)
        self.end_phase()

    def gla(self, li):
        P = self.P
        d = self.Ld[li]
        sp = self.spar
        C8, C9, C10, C11, C13 = 8, 9, 10, 11, 13
        yf = self.yflat
        cst = self.cst
        triT = cst.t[:, 0:128]
        uT = cst.t[:, 128:256]
        glam = cst.t[:, 256:384]
        aT = self.palloc("gla_aT", [17, T], F32)
        wal = self.palloc("gla_wal", [17, 512], F32)
        wina = self.palloc("gla_wina", [128, 256], BF16)
        P.dma("sp", wal.t[:], d["wal"].t[:, :], wal, wal)
        P.dma("pool", wina.t[:], d["wina"].t[:, :], wina, wina)
        P.op("dve", lambda h: h.memset(aT.t[:], 1.0), writes=[aT])
        for tg in range(4):
            bank = self.banks[tg % 2]
            for kc in range(NCH):
                P.op("pe", lambda h: h.matmul(
                    bank.t[0:16, :], wina.t[:, kc * 16:(kc + 1) * 16], self.hT_t[:, kc, tg * 512:(tg + 1) * 512],
                    start=(kc == 0), stop=(kc == NCH - 1)), reads=[wina, self.hT[kc]], writes=[bank])
            P.op("act", lambda h: h.activation(out=aT.t[0:16, tg * 512:(tg + 1) * 512], in_=bank.t[0:16, :],
                                               func=AF.Copy), reads=[bank], writes=[aT])
        S = self.palloc("gla_S", [128, 256], F32)
        Sb = [self.palloc(f"gla_Sb{i}", [128, 256], BF16) for i in range(2)]
        dec = self.palloc("gla_dec", [128, 32], F32)
        for hd in range(4):
            wq = self.load_w(d["win"].t[0 + hd, :, :])
            wk_ = self.load_w(d["win"].t[4 + hd, :, :])
            wv0 = self.load_w(d["win"].t[8 + 2 * hd, :, :])
            wv1 = self.load_w(d["win"].t[9 + 2 * hd, :, :])
            wgc = [self.load_w(d["win"].t[16 + 2 * hd + vc, :, :]) for vc in range(2)]
            P.op("dve", lambda h: h.memset(S.t[:], 0.0), writes=[S])
            P.op("dve", lambda h: h.memset(Sb[0].t[:], 0.0), writes=[Sb[0]])
            for tg in range(4):
                sl = slice(tg * 512, (tg + 1) * 512)
                b2 = self.banks[2]
                for ti in range(4):
                    tk0 = tg * 512 + ti * 128
                    P.op("pe", lambda h: h.matmul(
                        b2.t[:, ti * 128:(ti + 1) * 128], aT.t[0:17, tk0:tk0 + 128], wal.t[0:17, hd * 128:(hd + 1) * 128],
                        start=(ti == 0), stop=(ti == 3)), reads=[aT, wal], writes=[b2])
                L = self.wk("gla_L", [128, 512], F32)
                P.op("act", lambda h: h.activation(out=L.t[:], in_=b2.t[:], func=AF.Exp, scale=-1.0),
                     reads=[b2], writes=[L])
                P.op("act", lambda h: h.activation(out=L.t[:], in_=L.t[:], func=AF.Ln, bias=self.oneb.t[:, 0:1]),
                     reads=[self.oneb], writes=[L])
                b3 = self.banks[3]
                for ti in range(4):
                    P.op("pe", lambda h: h.matmul(
                        b3.t[:, ti * 128:(ti + 1) * 128], L.t[:, ti * 128:(ti + 1) * 128], triT,
                        start=(ti == 0), stop=(ti == 3)), reads=[L, cst], writes=[b3])
                Eb = self.wk("gla_Eb", [128, 512], F32)
                Enb = self.wk("gla_Enb", [128, 512], F32)
                P.op("act", lambda h: h.activation(out=Eb.t[:], in_=b3.t[:], func=AF.Exp), reads=[b3], writes=[Eb])
                P.op("act", lambda h: h.activation(out=Enb.t[:], in_=b3.t[:], func=AF.Exp, scale=-1.0),
                     reads=[b3], writes=[Enb])
                P.op("dve", lambda h: h.tensor_copy(out=dec.t[:, tg * 8:(tg + 1) * 8], in_=Eb.t[:, 63:512:64]),
                     reads=[Eb], writes=[dec])
                for ti in range(4):
                    P.op("pe", lambda h: h.matmul(
                        b2.t[:, ti * 128:(ti + 1) * 128], uT, L.t[:, ti * 128:(ti + 1) * 128],
                        start=(ti == 0), stop=(ti == 3)), reads=[L, cst], writes=[b2])
                Ed = self.wk("gla_Ed", [128, 512], F32)
                P.op("act", lambda h: h.activation(out=Ed.t[:], in_=b2.t[:], func=AF.Exp), reads=[b2], writes=[Ed])
                bq = self.banks[0]
                self.proj_fm(wq, tg, bq)
                P.op("dve", lambda h: h.scalar_tensor_tensor(
                    out=self.yT_t[:, C8, sl], in0=bq.t[:], scalar=128.0 ** -0.5, in1=Eb.t[:],
                    op0=ALU.mult, op1=ALU.mult), reads=[bq, Eb], writes=[self.yT[C8]])
                bk = self.banks[1]
                self.proj_fm(wk_, tg, bk)
                P.op("dve", lambda h: h.tensor_tensor(
                    out=self.yT_t[:, C9, sl], in0=bk.t[:], in1=Enb.t[:], op=ALU.mult),
                    reads=[bk, Enb], writes=[self.yT[C9]])
                bkt = self.banks[0]
                for ti in range(4):
                    tk0 = tg * 512 + ti * 128
                    self._tm_first = (ti == 0)
                    self._tm_last = (ti == 3)
                    self.proj_tm(wk_, bkt, slice(ti * 128, (ti + 1) * 128), slice(tk0, tk0 + 128))
                P.op("dve", lambda h: h.tensor_tensor(
                    out=self.yT_t[:, C10, sl], in0=bkt.t[:], in1=Ed.t[:], op=ALU.mult),
                    reads=[bkt, Ed], writes=[self.yT[C10]])
                for half in range(2):
                    bv = self.banks[1] if half == 0 else self.banks[0]
                    for t2 in range(2):
                        ti = half * 2 + t2
                        tk0 = tg * 512 + ti * 128
                        for vc, wv in enumerate((wv0, wv1)):
                            self._tm_first = (t2 == 0 and vc == 0)
                            self._tm_last = (t2 == 1 and vc == 1)
                            self.proj_tm(wv, bv, slice(t2 * 256 + vc * 128, t2 * 256 + (vc + 1) * 128),
                                         slice(tk0, tk0 + 128))
                    o0 = C11 * T + (tg * 4 + half * 2) * 256
                    P.op("act", lambda h: h.activation(out=yf[:, o0:o0 + 512], in_=bv.t[:], func=AF.Copy),
                         reads=[bv], writes=[self.yT[C11], self.yT[C11 + 1]])
                for vc in range(2):
                    bg = self.banks[vc]
                    self.proj_fm(wgc[vc], tg, bg)
                    P.op("act", lambda h: h.activation(out=self.yT_t[:, C13 + vc, sl], in_=bg.t[:], func=AF.Silu),
                         reads=[bg], writes=[self.yT[C13 + vc]])
                ob = [self.banks[4], self.banks[5]]
                for ti in range(4):
                    tile = tg * 4 + ti
                    tk0 = tile * 128
                    b6 = self.banks[6]
                    P.op("pe", lambda h: h.matmul(b6.t[:, 0:128], self.yT_t[:, C9, tk0:tk0 + 128],
                                                  self.yT_t[:, C8, tk0:tk0 + 128], start=True, stop=True),
                         reads=[self.yT[C8], self.yT[C9]], writes=[b6])
                    attm = self.wk("gla_attm", [128, 128], BF16)
                    P.op("dve", lambda h: h.tensor_tensor(out=attm.t[:], in0=b6.t[:, 0:128], in1=glam, op=ALU.mult),
                         reads=[b6, cst], writes=[attm])
                    for half in range(2):
                        c = tile * 2 + half
                        q0 = c * 64
                        sbc = Sb[c % 2]
                        sbn = Sb[(c + 1) % 2]
                        for vc in range(2):
                            P.op("pe", lambda h: h.matmul(
                                ob[vc].t[:, ti * 128 + half * 64: ti * 128 + half * 64 + 64],
                                sbc.t[:, vc * 128:(vc + 1) * 128], self.yT_t[:, C8, q0:q0 + 64],
                                start=(ti == 0 and half == 0), stop=False),
                                reads=[sbc, self.yT[C8]], writes=[ob[vc]])
                        b7 = self.banks[7]
                        kd0 = C10 * T + tile * 128
                        v0 = C11 * T + tile * 256
                        ps_ = slice(half * 64, (half + 1) * 64)
                        P.op("pe", lambda h: h.matmul(
                            b7.t[:, 0:256], yf[ps_, kd0:kd0 + 128], yf[ps_, v0:v0 + 256], start=True, stop=True),
                            reads=[self.yT[C10], self.yT[C11], self.yT[C11 + 1]], writes=[b7])
                        P.op("dve", lambda h: h.scalar_tensor_tensor(
                            out=S.t[:], in0=S.t[:], scalar=dec.t[:, c:c + 1], in1=b7.t[:, 0:256],
                            op0=ALU.mult, op1=ALU.add), reads=[dec, b7], writes=[S])
                        P.op("act", lambda h: h.activation(out=sbn.t[:], in_=S.t[:], func=AF.Copy),
                             reads=[S], writes=[sbn])
                    for vc in range(2):
                        vv = C11 * T + tile * 256 + vc * 128
                        P.op("pe", lambda h: h.matmul(
                            ob[vc].t[:, ti * 128:(ti + 1) * 128], yf[:, vv:vv + 128], attm.t[:],
                            start=False, stop=(ti == 3)),
                            reads=[self.yT[C11], self.yT[C11 + 1], attm], writes=[ob[vc]])
                b3 = self.banks[3]
                for vc in range(2):
                    sq = self.wk("gla_sq", [128, 512], BF16)
                    P.op("act", lambda h: h.activation(out=sq.t[:], in_=ob[vc].t[:], func=AF.Square),
                         reads=[ob[vc]], writes=[sq])
                    P.op("pe", lambda h: h.matmul(b3.t[:], self.ones.t[:], sq.t[:], start=(vc == 0), stop=(vc == 1)),
                         reads=[self.ones, sq], writes=[b3])
                rs = self.wk("gla_rs", [128, 512], F32)
                P.op("act", lambda h: h.activation(out=rs.t[:], in_=b3.t[:], func=AF.Sqrt, scale=1.0 / 256.0,
                                                   bias=self.epsb.t[:, 0:1]), reads=[b3, self.epsb], writes=[rs])
                P.op("dve", lambda h: h.reciprocal(out=rs.t[:], in_=rs.t[:]), reads=[], writes=[rs])
                for vc in range(2):
                    t1 = self.wk("gla_t1", [128, 512], F32)
                    hn = sp.t[:, 16 + hd * 2 + vc:16 + hd * 2 + vc + 1]
                    P.op("dve", lambda h: h.scalar_tensor_tensor(
                        out=t1.t[:], in0=ob[vc].t[:], scalar=hn, in1=rs.t[:], op0=ALU.mult, op1=ALU.mult),
                        reads=[ob[vc], sp, rs], writes=[t1])
                    P.op("pool", lambda h: h.tensor_tensor(
                        out=self.yT_t[:, 2 * hd + vc, sl], in0=t1.t[:], in1=self.yT_t[:, C13 + vc, sl], op=ALU.mult),
                        reads=[t1, self.yT[C13 + vc]], writes=[self.yT[2 * hd + vc]])

    def sgu(self, li):
        P = self.P
        d = self.Ld[li]
        sp = self.spar
        cst = self.cst
        wsf = self.palloc("sgu_wsf", [128, 512], F32)
        wm = self.palloc("sgu_wm", [128, 512], BF16)
        bsb = self.palloc("sgu_bsb", [128, 512], F32)
        bsr = self.palloc("sgu_bsr", [128, 512], F32)
        P.dma("sp", wsf.t[:], d["wsT"].t[:, :], wsf, wsf)
        P.dma("sp", bsb.t[:], d["bsb"].t[:, :], bsb, bsb)
        for g in range(4):
            P.op("dve", lambda h: h.tensor_tensor(out=wm.t[:, g * 128:(g + 1) * 128], in0=wsf.t[:, g * 128:(g + 1) * 128],
                                                  in1=cst.t[:, 384:512], op=ALU.mult), reads=[wsf, cst], writes=[wm])
        m = self.palloc("sgu_m", [128, 2, T], BF16)
        vn = self.palloc("sgu_vn", [128, 16, 256], BF16)
        for g in range(4):
            for ti in range(4):
                P.op("dve", lambda h: h.tensor_copy(out=bsr.t[:, ti * 128:(ti + 1) * 128],
                                                    in_=bsb.t[:, g * 128:(g + 1) * 128]), reads=[bsb], writes=[bsr])
            for ec in range(2):
                wu = self.load_w(d["win"].t[24 + 2 * g + ec, :, :])
                wgd = self.load_w(d["win"].t[40 + 2 * g + ec, :, :])
                for tg in range(4):
                    sl = slice(tg * 512, (tg + 1) * 512)
                    b0, b1 = self.banks[0], self.banks[1]
                    self.proj_fm(wu, tg, b0)
                    self.proj_fm(wgd, tg, b1)
                    gu = self.wk("sgu_gu", [128, 512], F32)
                    sg = self.wk("sgu_sg", [128, 512], F32)
                    P.op("act", lambda h: h.activation(out=gu.t[:], in_=b0.t[:], func=AF.Gelu_apprx_tanh),
                         reads=[b0], writes=[gu])
                    P.op("act", lambda h: h.activation(out=sg.t[:], in_=b1.t[:], func=AF.Silu),
                         reads=[b1], writes=[sg])
                    P.op("pool", lambda h: h.tensor_tensor(out=m.t[:, ec, sl], in0=gu.t[:], in1=sg.t[:], op=ALU.mult),
                         reads=[gu, sg], writes=[m])
            wv = [self.load_w(d["win"].t[32 + 2 * g + ec, :, :]) for ec in range(2)]
            for tile in range(16):
                bank = self.banks[2 + tile % 2]
                tk0 = tile * 128
                for ec in range(2):
                    self._tm_first = (ec == 0)
                    self._tm_last = (ec == 1)
                    self.proj_tm(wv[ec], bank, slice(ec * 128, (ec + 1) * 128), slice(tk0, tk0 + 128))
                gv = self.wk("sgu_gv", [128, 256], F32)
                P.op("act", lambda h: h.activation(out=gv.t[:], in_=bank.t[:, 0:256], func=AF.Gelu_apprx_tanh),
                     reads=[bank], writes=[gv])
                st6 = self.wk("sgu_st6", [128, 6], F32)
                mv = self.wk("sgu_mv", [128, 2], F32)
                P.op("dve", lambda h: h.bn_stats(out=st6.t[:], in_=gv.t[:]), reads=[gv], writes=[st6])
                P.op("dve", lambda h: h.bn_aggr(out=mv.t[:], in_=st6.t[:]), reads=[st6], writes=[mv])
                sd = self.wk("sgu_sd", [128, 1], F32)
                P.op("act", lambda h: h.activation(out=sd.t[:], in_=mv.t[:, 1:2], func=AF.Sqrt,
                                                   bias=self.epsb.t[:, 0:1]), reads=[mv, self.epsb], writes=[sd])
                P.op("dve", lambda h: h.reciprocal(out=sd.t[:], in_=sd.t[:]), reads=[], writes=[sd])
                P.op("dve", lambda h: h.tensor_scalar(out=vn.t[:, tile, :], in0=gv.t[:], scalar1=mv.t[:, 0:1],
                                                      scalar2=sd.t[:, 0:1], op0=ALU.subtract, op1=ALU.mult),
                     reads=[gv, mv, sd], writes=[vn])
            for ec in range(2):
                for tg in range(4):
                    sl = slice(tg * 512, (tg + 1) * 512)
                    bank = self.banks[4 + (ec * 4 + tg) % 2]
                    for ti in range(4):
                        tile = tg * 4 + ti
                        P.op("pe", lambda h: h.matmul(
                            bank.t[:, ti * 128:(ti + 1) * 128], vn.t[:, tile, ec * 128:(ec + 1) * 128],
                            wm.t[:, g * 128:(g + 1) * 128], start=(ti == 0), stop=(ti == 3)),
                            reads=[vn, wm], writes=[bank])
                    t1 = self.wk("sgu_t1", [128, 512], F32)
                    vnm = sp.t[:, 24 + g * 2 + ec:24 + g * 2 + ec + 1]
                    P.op("dve", lambda h: h.scalar_tensor_tensor(
                        out=t1.t[:], in0=bank.t[:], scalar=vnm, in1=bsr.t[:], op0=ALU.mult, op1=ALU.add),
                        reads=[bank, sp, bsr], writes=[t1])
                    P.op("pool", lambda h: h.tensor_tensor(
                        out=self.yT_t[:, 8 + 2 * g + ec, sl], in0=t1.t[:], in1=m.t[:, ec, sl], op=ALU.mult),
                        reads=[t1, m], writes=[self.yT[8 + 2 * g + ec]])

    def build(self):
        nl = len(self.layers)
        self.load_params(0)
        self.first_norm()
        for li, kind in enumerate(self.layers):
            if kind == "e":
                self.even_layer(li)
            else:
                self.odd_layer(li)
            last = (li == nl - 1)
            if not last:
                pass
            self.out_phase(li, last)
            if not last:
                self.load_params(li + 1)
        return self.nc


def _tile_w(w, ncol_chunks=None):
    K, C = w.shape
    cc = C // 128
    a = w.reshape(K // 128, 128, cc, 128)
    a = np.ascontiguousarray(a.transpose(2, 1, 0, 3))
    return a.reshape(cc, 128, (K // 128) * 128)


def _pp(v):
    return np.ascontiguousarray(v.reshape(-1, 128).T)


def _consts():
    s = np.arange(128)[:, None]
    t = np.arange(128)[None, :]
    same = (s // 64) == (t // 64)
    triT = np.where(same & (s <= t), -1.0 / 16.0, 0.0)
    uT = np.where(same & (s > t), -1.0 / 16.0, 0.0)
    glam = np.where(same & (s <= t), 1.0, 0.0)
    sgm = np.where(s <= t, 1.0, 0.0)
    cst = np.concatenate([triT, uT, glam, sgm], axis=1).astype(np.float32)
    am = np.zeros((4, 128, 3, 256), np.float64)
    k = np.arange(128)[:, None].astype(np.float64)
    q = np.arange(128)[None, :].astype(np.float64)
    for g in range(3):
        for j in range(4):
            slope = 2.0 ** (-8.0 * (g + 3 * j + 1) / 12.0)
            dil = DIL[g]
            prev = np.where(k >= q, np.exp(-slope * dil * (q + 128 - k)), 0.0)
            cur = np.where(k <= q, np.exp(-slope * dil * (q - k)), 0.0)
            am[j, :, g, 0:128] = prev
            am[j, :, g, 128:256] = cur
    return cst, am.reshape(4, 128, 768).astype(np.float32)


LAYERS = ["e", "o", "e", "o"]


def prep_shared(inp, layers):
    f = lambda a: np.ascontiguousarray(np.asarray(a, dtype=np.float32))
    m = {}
    cst, am = _consts()
    m["cst"] = cst
    m["amask"] = am
    m["fg"] = _pp(f(inp["final_norm"]))
    ie = io = 0
    for li, kind in enumerate(layers):
        if kind == "e":
            i = ie
            ie += 1
            m[f"L{li}_win"] = _tile_w(f(inp["ev_w_in"][i]))
            m[f"L{li}_wout"] = _tile_w(f(inp["ev_w_out"][i]))
            sp = np.zeros((128, 112), np.float32)
            sp[:, 0:16] = _pp(f(inp["ev_norm"][i]))
            cw = f(inp["ev_conv_w"][i])
            sp[:, 16:64] = cw.reshape(4, 12, 128).transpose(2, 1, 0).reshape(128, 48)
            sp[:, 64:76] = _pp(f(inp["ev_conv_b"][i]))
            sp[:, 76:88] = _pp(f(inp["ev_lam"][i]))
            sp[:, 88:100] = _pp(f(inp["ev_b_a"][i]).reshape(-1))
            sp[:, 100:112] = _pp(f(inp["ev_b_i"][i]).reshape(-1))
            m[f"L{li}_sp"] = sp
            wa = f(inp["ev_w_a"][i]).transpose(1, 0, 2).reshape(128, 1536)
            wi = f(inp["ev_w_i"][i]).transpose(1, 0, 2).reshape(128, 1536)
            m[f"L{li}_wg"] = np.ascontiguousarray(np.concatenate([wa, wi], axis=1))
        else:
            i = io
            io += 1
            w = f(inp["od_w_in"][i])
            cols = np.concatenate([np.arange(0, 2048), np.arange(2064, 6160)])
            m[f"L{li}_win"] = _tile_w(np.ascontiguousarray(w[:, cols]))
            wa = w[:, 2048:2064].reshape(16, 128, 16).transpose(1, 0, 2).reshape(128, 256)
            m[f"L{li}_wina"] = np.ascontiguousarray(wa)
            m[f"L{li}_wout"] = _tile_w(f(inp["od_w_out"][i]))
            sp = np.zeros((128, 32), np.float32)
            sp[:, 0:16] = _pp(f(inp["od_norm"][i]))
            sp[:, 16:24] = _pp(f(inp["od_head_norm"][i]).reshape(-1))
            sp[:, 24:32] = _pp(f(inp["od_v_norm"][i]).reshape(-1))
            m[f"L{li}_sp"] = sp
            m[f"L{li}_wal"] = np.ascontiguousarray(
                np.concatenate([f(inp["od_w_alpha"][i]), f(inp["od_b_alpha"][i])[None, :]], axis=0))
            ws = f(inp["od_w_s"][i])
            m[f"L{li}_wsT"] = np.ascontiguousarray(ws.transpose(2, 0, 1).reshape(128, 512))
            bs = f(inp["od_b_s"][i]).reshape(1, 512)
            m[f"L{li}_bsb"] = np.ascontiguousarray(np.broadcast_to(bs, (128, 512)))
    return m


def run_layers(x, inp, layers, final=True):
    b = Builder(layers, final=final)
    nc = b.build()
    shared = prep_shared(inp, layers)
    in_maps = []
    for core in range(N_CORES):
        bi = core % 4
        xt = np.ascontiguousarray(x[bi].T).reshape(NCH, 128, T)
        mm = dict(shared)
        mm["xin"] = xt
        in_maps.append(mm)
    res = run_bass_kernel_spmd(nc, in_maps, core_ids=list(range(N_CORES)))
    out = np.empty((4, T, D), np.float32)
    for bi in range(4):
        out[bi] = res.results[bi]["yout"].reshape(D, T).T
    return out


def kernel(**inputs):
    x = np.asarray(inputs["x"], dtype=np.float32)
    return run_layers(x, inputs, LAYERS, final=True)
```

```python
import numpy as np
import concourse.bass as bass
import concourse.mybir as mybir
from concourse.bass_utils import run_bass_kernel_spmd

F32 = mybir.dt.float32
BF16 = mybir.dt.bfloat16
AF = mybir.ActivationFunctionType
ALU = mybir.AluOpType

D = 2048
T = 2048
NCH = 16
EPS = 1e-6
SEM_LIMIT = 16000
SAME_ENG_SYNC = True
N_CORES = 8

DIL = (1, 4, 16)


class Tk:
    __slots__ = ("name", "w", "r", "dsem", "dcnt", "t")

    def __init__(self, name, t=None):
        self.name = name
        self.w = None
        self.r = {}
        self.dsem = None
        self.dcnt = 0
        self.t = t


class Eng:
    def __init__(self, name, h):
        self.name = name
        self.h = h
        self.sem = None
        self.cnt = 0
        self.known = {}
        self.nsem = 0


class Prog:
    def __init__(self):
        self.nc = bass.Bass("TRN2", target_bir_lowering=False)
        nc = self.nc
        self.E = {
            "pe": Eng("pe", nc.tensor),
            "act": Eng("act", nc.scalar),
            "dve": Eng("dve", nc.vector),
            "pool": Eng("pool", nc.gpsimd),
            "sp": Eng("sp", nc.sync),
        }
        self.n_inst = 0
        self.n_wait = 0
        self.dma_tks = {}
        self.allsems = []
        self.sem_pool = []
        self.swdge_sems = set()

    def sb(self, name, shape, dt):
        t = self.nc.alloc_sbuf_tensor(name, list(shape), dt)
        return Tk(name, t)

    def ps(self, name):
        t = self.nc.alloc_psum_tensor(name, [128, 512], F32)
        return Tk(name, t)

    def dram(self, name, shape, dt, kind):
        t = self.nc.dram_tensor(name, list(shape), dt, kind=kind)
        return Tk(name, t.ap())

    def _wait(self, e, evs):
        for (sem, val) in evs:
            k = id(sem)
            if e.known.get(k, 0) >= val:
                continue
            e.h.wait_ge(sem, val)
            e.known[k] = val
            self.n_wait += 1

    def _deps(self, e, reads, writes):
        evs = {}

        def add(ev):
            if ev is None:
                return
            k = id(ev[0])
            if k not in evs or evs[k][1] < ev[1]:
                evs[k] = ev

        for t in reads:
            add(t.w)
        for t in writes:
            add(t.w)
            for ev in t.r.values():
                add(ev)
        out = []
        for ev in evs.values():
            if ev[0] is e.sem:
                if e.name == "pe" or (e.name != "pool" and not SAME_ENG_SYNC):
                    continue
            out.append(ev)
        return out

    def _newsem(self, e):
        if e.sem is None or e.cnt >= SEM_LIMIT:
            e.sem = self.nc.alloc_semaphore(f"s_{e.name}_{e.nsem}")
            self.allsems.append(e.sem)
            e.nsem += 1
            e.cnt = 0

    def op(self, en, fn, reads=(), writes=()):
        e = self.E[en]
        self._newsem(e)
        self._wait(e, self._deps(e, reads, writes))
        inst = fn(e.h)
        e.cnt += 1
        inst.then_inc(e.sem, 1)
        ev = (e.sem, e.cnt)
        for t in reads:
            t.r[id(e.sem)] = ev
        for t in writes:
            t.w = ev
            t.r = {}
        self.n_inst += 1
        return inst

    def dma(self, qn, out, in_, dst, semtk, reads=(), **kw):
        e = self.E[qn]
        if semtk.dsem is None:
            if self.sem_pool and qn != "pool":
                semtk.dsem, semtk.dcnt = self.sem_pool.pop()
            else:
                semtk.dsem = self.nc.alloc_semaphore(f"d{len(self.allsems)}_{semtk.name}")
                self.allsems.append(semtk.dsem)
        if qn == "pool":
            self.swdge_sems.add(id(semtk.dsem))
        self.dma_tks[id(semtk)] = semtk
        self._wait(e, self._deps(e, reads, (dst,)))
        inst = e.h.dma_start(out=out, in_=in_, **kw)
        semtk.dcnt += 16
        inst.then_inc(semtk.dsem, 16)
        ev = (semtk.dsem, semtk.dcnt)
        for t in reads:
            t.r[id(semtk.dsem)] = ev
        dst.w = ev
        dst.r = {}
        self.n_inst += 1
        return inst

    def wait_all(self, en, tks):
        e = self.E[en]
        evs = []
        for t in tks:
            if t.w is not None:
                evs.append(t.w)
        self._wait(e, evs)

    def barrier(self):
        evs = []
        for f in self.E.values():
            if f.sem is not None and f.cnt > 0:
                evs.append((f.sem, f.cnt))
        for t in self.dma_tks.values():
            if t.dcnt > 0:
                evs.append((t.dsem, t.dcnt))
        for e in self.E.values():
            self._wait(e, evs)


class Builder:
    def __init__(self, layers, final=True):
        self.P = Prog()
        P = self.P
        self.layers = layers
        self.n_lru = 12
        self.final = final
        nc = P.nc
        self.nc = nc
        self.xin = P.dram("xin", [NCH, 128, T], F32, "ExternalInput")
        self.yout = P.dram("yout", [NCH, 128, T], F32, "ExternalOutput")
        self.youts = [[Tk(f"yo{c}_{g}") for g in range(4)] for c in range(NCH)]
        self.xs_ap = nc.dram_tensor("xs", [NCH, 128, T], F32, kind="Internal").ap()
        self.xs = [[Tk(f"xs{c}_{g}") for g in range(4)] for c in range(NCH)]
        self.cst_d = P.dram("cst", [128, 4 * 128], F32, "ExternalInput")
        self.amask_d = P.dram("amask", [4, 128, 3 * 256], F32, "ExternalInput")
        self.fg_d = P.dram("fg", [128, NCH], F32, "ExternalInput")
        self.Ld = []
        for li, kind in enumerate(layers):
            d = {}
            if kind == "e":
                d["win"] = P.dram(f"L{li}_win", [64, 128, 2048], F32, "ExternalInput")
                d["wout"] = P.dram(f"L{li}_wout", [16, 128, 2048], F32, "ExternalInput")
                d["sp"] = P.dram(f"L{li}_sp", [128, 112], F32, "ExternalInput")
                d["wg"] = P.dram(f"L{li}_wg", [128, 2 * 12 * 128], F32, "ExternalInput")
            else:
                d["win"] = P.dram(f"L{li}_win", [48, 128, 2048], F32, "ExternalInput")
                d["wina"] = P.dram(f"L{li}_wina", [128, 256], F32, "ExternalInput")
                d["wout"] = P.dram(f"L{li}_wout", [16, 128, 2048], F32, "ExternalInput")
                d["sp"] = P.dram(f"L{li}_sp", [128, 32], F32, "ExternalInput")
                d["wal"] = P.dram(f"L{li}_wal", [17, 512], F32, "ExternalInput")
                d["wsT"] = P.dram(f"L{li}_wsT", [128, 512], F32, "ExternalInput")
                d["bsb"] = P.dram(f"L{li}_bsb", [128, 512], F32, "ExternalInput")
            self.Ld.append(d)

        self.hT_t = nc.alloc_sbuf_tensor("hT", [128, NCH, T], BF16)
        self.hT = [Tk(f"hT{c}", self.hT_t) for c in range(NCH)]
        self.yT_t = nc.alloc_sbuf_tensor("yT", [128, NCH, T], BF16)
        self.yT = [Tk(f"yT{c}", self.yT_t) for c in range(NCH)]
        self.yflat = self.yT_t[:, :, :].rearrange("p c t -> p (c t)")
        self.NSLOT = 6
        self.wslot = [P.sb(f"wslot{i}", [128, NCH * 128], BF16) for i in range(self.NSLOT)]
        self.wnext = 0
        self.ones = P.sb("ones", [128, 128], BF16)
        self.cst = P.sb("cstsb", [128, 512], F32)
        self.spar = P.sb("spar", [128, 112], F32)
        self.sparn = P.sb("sparn", [128, 16], F32)
        self.fg = P.sb("fgsb", [128, NCH], F32)
        self.epsb = P.sb("epsb", [128, 1], F32)
        self.oneb = P.sb("oneb", [128, 1], F32)
        self.banks = [P.ps(f"bank{i}") for i in range(8)]
        self._wk = {}
        self._guards = []
        self._ptks = []
        self._pid = 0

        P.op("dve", lambda h: h.memset(self.ones.t[:], 1.0), writes=[self.ones])
        P.op("dve", lambda h: h.memset(self.epsb.t[:], EPS), writes=[self.epsb])
        P.op("dve", lambda h: h.memset(self.oneb.t[:], 1.0), writes=[self.oneb])
        P.dma("sp", self.cst.t[:], self.cst_d.t[:, :], self.cst, self.cst)
        P.dma("sp", self.fg.t[:], self.fg_d.t[:, :], self.fg, self.fg)

    def palloc(self, name, shape, dt):
        g = self.nc.sbuf_tensor(f"{name}_p{self._pid}", list(shape), dt)
        t = g.__enter__()
        self._guards.append(g)
        tk = Tk(name, t)
        self._ptks.append(tk)
        return tk

    def end_phase(self):
        self.P.barrier()
        for g in reversed(self._guards):
            g.__exit__(None, None, None)
        self._guards = []
        for tk in self._ptks:
            if tk.dsem is not None:
                if id(tk.dsem) not in self.P.swdge_sems:
                    self.P.sem_pool.append((tk.dsem, tk.dcnt))
                self.P.dma_tks.pop(id(tk), None)
                tk.dsem = None
        self._ptks = []
        self._wk = {}
        self._pid += 1

    def wk(self, name, shape, dt, n=2):
        if name not in self._wk:
            self._wk[name] = [[self.palloc(f"{name}_{i}", shape, dt) for i in range(n)], 0]
        lst = self._wk[name]
        t = lst[0][lst[1] % n]
        lst[1] += 1
        return t

    def load_w(self, src_ap):
        P = self.P
        s = self.wslot[self.wnext % self.NSLOT]
        self.wnext += 1
        P.dma("pool", s.t[:, :], src_ap, s, s)
        return s

    def proj_fm(self, ws, tg, bank, nrow=128):
        P = self.P
        for kc in range(NCH):
            P.op("pe", lambda h: h.matmul(
                bank.t[0:nrow, :], ws.t[:, kc * nrow:(kc + 1) * nrow], self.hT_t[:, kc, tg * 512:(tg + 1) * 512],
                start=(kc == 0), stop=(kc == NCH - 1)),
                reads=[ws, self.hT[kc]], writes=[bank])

    def proj_tm(self, ws, bank, osl, tsl):
        P = self.P
        for kc in range(NCH):
            P.op("pe", lambda h: h.matmul(
                bank.t[:, osl], self.hT_t[:, kc, tsl], ws.t[:, kc * 128:(kc + 1) * 128],
                start=(kc == 0 and self._tm_first), stop=(kc == NCH - 1 and self._tm_last)),
                reads=[ws, self.hT[kc]], writes=[bank])

    def norm_stats_step(self, xt, c, tg, ssq, gain_tk, write_h, defer=False):
        P = self.P
        sl = slice(tg * 512, (tg + 1) * 512)
        sq = self.wk("sq", [128, 512], BF16, n=3)
        P.op("act", lambda h: h.activation(out=sq.t[:], in_=xt.t[:], func=AF.Square), reads=[xt], writes=[sq])

        def emit_ssq():
            P.op("pe", lambda h: h.matmul(ssq[tg].t[:], self.ones.t[:], sq.t[:], start=(c == 0), stop=(c == NCH - 1)),
                 reads=[self.ones, sq], writes=[ssq[tg]])

        if write_h:
            P.op("act", lambda h: h.activation(
                out=self.hT_t[:, c, sl], in_=xt.t[:], func=AF.Copy, scale=gain_tk.t[:, c:c + 1]),
                reads=[xt, gain_tk], writes=[self.hT[c]])
        if defer:
            return emit_ssq
        emit_ssq()
        return None

    def first_norm(self):
        P = self.P
        ssq = self.banks[4:8]
        for c in range(NCH):
            for tg in range(4):
                sl = slice(tg * 512, (tg + 1) * 512)
                xt = self.wk("xt", [128, 512], F32, n=3)
                P.dma("sp", xt.t[:], self.xin.t[c, :, sl], xt, xt)
                self.norm_stats_step(xt, c, tg, ssq, self.spar, True)
        self.finish_norm(ssq)
        self.scale_h()
        self.end_phase()

    def finish_norm(self, ssq):
        P = self.P
        self.rstd = self.palloc("rstd", [128, T], F32)
        for tg in range(4):
            sl = slice(tg * 512, (tg + 1) * 512)
            tmp = self.wk("nrm_tmp", [128, 512], F32)
            P.op("act", lambda h: h.activation(out=tmp.t[:], in_=ssq[tg].t[:], func=AF.Sqrt,
                                               scale=1.0 / D, bias=self.epsb.t[:, 0:1]),
                 reads=[ssq[tg], self.epsb], writes=[tmp])
            P.op("dve", lambda h: h.reciprocal(out=self.rstd.t[:, sl], in_=tmp.t[:]),
                 reads=[tmp], writes=[self.rstd])

    def scale_h(self):
        P = self.P
        for c in range(NCH):
            P.op("dve", lambda h: h.tensor_tensor(out=self.hT_t[:, c, :], in0=self.hT_t[:, c, :],
                                               in1=self.rstd.t[:], op=ALU.mult),
                 reads=[self.rstd], writes=[self.hT[c]])

    def out_phase(self, li, last):
        P = self.P
        d = self.Ld[li]
        ssq = self.banks[4:8]
        if not last:
            P.dma("sp", self.sparn.t[:, 0:16], self.Ld[li + 1]["sp"].t[:, 0:16], self.sparn, self.sparn)
        pending = None
        steps = [(dc, tg) for dc in range(NCH) for tg in range(4)]
        xts = {}
        PF = 2

        def issue_load(k):
            dc_, tg_ = steps[k]
            sl_ = slice(tg_ * 512, (tg_ + 1) * 512)
            x_ = self.wk("xt", [128, 512], F32, n=4)
            if li == 0:
                P.dma("sp", x_.t[:], self.xin.t[dc_, :, sl_], x_, x_)
            else:
                P.dma("sp", x_.t[:], self.xs_ap[dc_, :, sl_], x_, x_, reads=[self.xs[dc_][tg_]])
            xts[k] = x_

        for k in range(PF):
            issue_load(k)
        for dc in range(NCH):
            ws = self.load_w(d["wout"].t[dc, :, :])
            for tg in range(4):
                sl = slice(tg * 512, (tg + 1) * 512)
                k = dc * 4 + tg
                if k + PF < len(steps):
                    issue_load(k + PF)
                xt = xts.pop(k)
                bank = self.banks[(dc * 4 + tg) % 2]
                for fc in range(NCH):
                    P.op("pe", lambda h: h.matmul(
                        bank.t[:], ws.t[:, fc * 128:(fc + 1) * 128], self.yT_t[:, fc, sl],
                        start=(fc == 0), stop=(fc == NCH - 1)),
                        reads=[ws, self.yT[fc]], writes=[bank])
                if pending is not None:
                    pending()
                P.op("dve", lambda h: h.tensor_tensor(out=xt.t[:], in0=bank.t[:], in1=xt.t[:], op=ALU.add),
                     reads=[bank], writes=[xt])
                pending = self.norm_stats_step(xt, dc, tg, ssq, self.sparn, not last, defer=True)
                P.dma("sp", self.xs_ap[dc, :, sl], xt.t[:], self.xs[dc][tg], xt, reads=[xt])
        if pending is not None:
            pending()
        self.finish_norm(ssq)
        if not last:
            self.scale_h()
        else:
            self.final_out()
        self.end_phase()

    def final_out(self):
        P = self.P
        for c in range(NCH):
            xf = self.wk("xf", [128, T], F32, n=2)
            P.dma("sp", xf.t[:], self.xs_ap[c, :, :], xf, xf, reads=[self.xs[c][tg] for tg in range(4)])
            if self.final:
                P.op("dve", lambda h: h.scalar_tensor_tensor(
                    out=xf.t[:], in0=xf.t[:], scalar=self.fg.t[:, c:c + 1], in1=self.rstd.t[:],
                    op0=ALU.mult, op1=ALU.mult),
                    reads=[self.fg, self.rstd], writes=[xf])
            P.dma("sp", self.yout.t[c, :, :], xf.t[:], self.youts[c][0], xf, reads=[xf])
        P.wait_all("sp", [self.youts[c][0] for c in range(NCH)])

    def load_params(self, li):
        P = self.P
        d = self.Ld[li]
        n = 112 if self.layers[li] == "e" else 32
        P.dma("sp", self.spar.t[:, 0:n], d["sp"].t[:, :], self.spar, self.spar)

    def even_layer(self, li):
        self.attention(li)
        self.end_phase()
        self.lru(li)
        self.end_phase()

    def lru(self, li):
        P = self.P
        d = self.Ld[li]
        sp = self.spar
        wg = self.palloc("wg", [128, 2 * 12 * 128], BF16)
        for q in range(2):
            P.dma("pool", wg.t[:, q * 1536:(q + 1) * 1536], d["wg"].t[:, q * 1536:(q + 1) * 1536], wg, wg)
        cc = self.palloc("lru_c", [128, 12], F32)
        ee = self.palloc("lru_e", [128, 12], F32)
        t1 = self.palloc("lru_t1", [128, 12], F32)
        P.op("act", lambda h: h.activation(out=ee.t[:], in_=sp.t[:, 76:88], func=AF.Exp, scale=-1.0),
             reads=[sp], writes=[ee])
        P.op("dve", lambda h: h.tensor_scalar(out=t1.t[:], in0=ee.t[:], scalar1=-1.0 / 3.0, scalar2=0.5,
                                              op0=ALU.mult, op1=ALU.add), reads=[ee], writes=[t1])
        P.op("dve", lambda h: h.tensor_tensor(out=t1.t[:], in0=t1.t[:], in1=ee.t[:], op=ALU.mult),
             reads=[ee], writes=[t1])
        P.op("dve", lambda h: h.tensor_scalar(out=t1.t[:], in0=t1.t[:], scalar1=-1.0, scalar2=1.0,
                                              op0=ALU.mult, op1=ALU.add), reads=[], writes=[t1])
        P.op("dve", lambda h: h.tensor_tensor(out=t1.t[:], in0=t1.t[:], in1=ee.t[:], op=ALU.mult),
             reads=[ee], writes=[t1])
        P.op("dve", lambda h: h.tensor_scalar(out=cc.t[:], in0=t1.t[:], scalar1=-8.0, scalar2=None, op0=ALU.mult),
             reads=[t1], writes=[cc])
        NH = self.n_lru
        steps = [(hh, tg) for hh in range(NH) for tg in range(4)]
        wts = {}
        xas = {}
        hprev = {}

        def stage_a(si):
            hh, tg = steps[si]
            if tg == 0:
                wts[hh] = (self.load_w(d["win"].t[hh, :, :]), self.load_w(d["win"].t[NH + hh, :, :]))
                xas[hh] = self.wk("lru_xa", [128, 4, 3 + 512], F32, n=2)
            w_xa, w_ga = wts[hh]
            xa = xas[hh]
            b0 = self.banks[0 + si % 2]
            b1 = self.banks[4 + si % 2]
            self.proj_fm(w_xa, tg, b0)
            self.proj_fm(w_ga, tg, b1)
            P.op("act", lambda h: h.activation(out=xa.t[:, tg, 3:515], in_=b0.t[:], func=AF.Copy),
                 reads=[b0], writes=[xa])
            if tg == 0:
                P.op("dve", lambda h: h.memset(xa.t[:, 0, 0:3], 0.0), writes=[xa])
            else:
                P.op("dve", lambda h: h.tensor_copy(out=xa.t[:, tg, 0:3], in_=xa.t[:, tg - 1, 512:515]),
                     writes=[xa])
            sg = self.wk("lru_sg", [128, 512], BF16, n=3)
            P.op("act", lambda h: h.activation(out=sg.t[:], in_=b1.t[:], func=AF.Silu), reads=[b1], writes=[sg])
            return sg

        def stage_b(si, sg):
            hh, tg = steps[si]
            xa = xas[hh]
            sl = slice(tg * 512, (tg + 1) * 512)
            cw = lambda j: sp.t[:, 16 + hh * 4 + j:16 + hh * 4 + j + 1]
            xc = self.wk("lru_xc", [128, 512], F32)
            P.op("dve", lambda h: h.tensor_scalar(
                out=xc.t[:], in0=xa.t[:, tg, 0:512], scalar1=cw(0), scalar2=sp.t[:, 64 + hh:65 + hh],
                op0=ALU.mult, op1=ALU.add), reads=[xa, sp], writes=[xc])
            for j in range(1, 4):
                P.op("dve", lambda h: h.scalar_tensor_tensor(
                    out=xc.t[:], in0=xa.t[:, tg, j:j + 512], scalar=cw(j), in1=xc.t[:],
                    op0=ALU.mult, op1=ALU.add), reads=[xa, sp], writes=[xc])
            xcb = self.wk("lru_xcb", [128, 512], BF16)
            P.op("act", lambda h: h.activation(out=xcb.t[:], in_=xc.t[:], func=AF.Copy), reads=[xc], writes=[xcb])
            br = self.banks[2 + 4 * (si % 2)]
            bi = self.banks[3 + 4 * (si % 2)]
            P.op("pe", lambda h: h.matmul(br.t[:], wg.t[:, hh * 128:(hh + 1) * 128], xcb.t[:],
                                          start=True, stop=True), reads=[wg, xcb], writes=[br])
            P.op("pe", lambda h: h.matmul(bi.t[:], wg.t[:, 1536 + hh * 128:1536 + (hh + 1) * 128], xcb.t[:],
                                          start=True, stop=True), reads=[wg, xcb], writes=[bi])
            rr = self.wk("lru_r", [128, 512], F32)
            ii = self.wk("lru_i", [128, 512], F32)
            P.op("act", lambda h: h.activation(out=rr.t[:], in_=br.t[:], func=AF.Sigmoid,
                                               bias=sp.t[:, 88 + hh:89 + hh]), reads=[br, sp], writes=[rr])
            P.op("act", lambda h: h.activation(out=ii.t[:], in_=bi.t[:], func=AF.Sigmoid,
                                               bias=sp.t[:, 100 + hh:101 + hh]), reads=[bi, sp], writes=[ii])
            aa = self.wk("lru_a", [128, 512], F32)
            P.op("act", lambda h: h.activation(out=aa.t[:], in_=rr.t[:], func=AF.Exp,
                                               scale=cc.t[:, hh:hh + 1]), reads=[rr, cc], writes=[aa])
            P.op("dve", lambda h: h.tensor_tensor(out=rr.t[:], in0=aa.t[:], in1=aa.t[:], op=ALU.mult),
                 reads=[aa], writes=[rr])
            P.op("act", lambda h: h.activation(out=rr.t[:], in_=rr.t[:], func=AF.Sqrt, scale=-1.0,
                                               bias=self.oneb.t[:, 0:1]), reads=[self.oneb], writes=[rr])
            P.op("dve", lambda h: h.tensor_tensor(out=ii.t[:], in0=ii.t[:], in1=xc.t[:], op=ALU.mult),
                 reads=[xc], writes=[ii])
            P.op("dve", lambda h: h.tensor_tensor(out=ii.t[:], in0=ii.t[:], in1=rr.t[:], op=ALU.mult),
                 reads=[rr], writes=[ii])
            hb = self.wk("lru_h", [128, 512], F32, n=3)
            hp = hprev.get(hh) if tg > 0 else None
            init = 0.0 if hp is None else hp.t[:, 511:512]
            rds = [aa, ii] + ([hp] if hp is not None else [])
            P.op("dve", lambda h: h.tensor_tensor_scan(
                out=hb.t[:], data0=aa.t[:], data1=ii.t[:], initial=init, op0=ALU.mult, op1=ALU.add),
                reads=rds, writes=[hb])
            hprev[hh] = hb
            P.op("pool", lambda h: h.tensor_tensor(
                out=self.yT_t[:, hh, sl], in0=hb.t[:], in1=sg.t[:], op=ALU.mult),
                reads=[hb, sg], writes=[self.yT[hh]])

        sgs = {0: stage_a(0)}
        for si in range(len(steps)):
            if si + 1 < len(steps):
                sgs[si + 1] = stage_a(si + 1)
            stage_b(si, sgs.pop(si))

    def attention(self, li):
        P = self.P
        d = self.Ld[li]
        scale = 128.0 ** -0.5
        for j in range(4):
            am = self.wk("amask", [128, 3 * 256], F32, n=2)
            P.dma("sp", am.t[:], self.amask_d.t[j, :, :], am, am)
            for g in range(3):
                dil = DIL[g]
                for base, dstc in ((24, g), (36, 3 + g)):
                    ws = self.load_w(d["win"].t[base + g * 4 + j, :, :])
                    for tg in range(4):
                        bank = self.banks[tg % 2]
                        self.proj_fm(ws, tg, bank)
                        if tg % 2 == 0:
                            P.op("act", lambda h: h.activation(
                                out=self.yT_t[:, dstc, tg * 512:(tg + 1) * 512], in_=bank.t[:], func=AF.Copy),
                                reads=[bank], writes=[self.yT[dstc]])
                        else:
                            P.op("dve", lambda h: h.tensor_copy(
                                out=self.yT_t[:, dstc, tg * 512:(tg + 1) * 512], in_=bank.t[:]),
                                reads=[bank], writes=[self.yT[dstc]])
                ws = self.load_w(d["win"].t[48 + g * 4 + j, :, :])
                nb = 16 // dil
                for blk4 in range(4):
                    bank = self.banks[blk4 % 2]
                    for b_ in range(4):
                        blk = blk4 * 4 + b_
                        r, n = blk // nb, blk % nb
                        t0 = r + dil * 128 * n
                        self._tm_first = (b_ == 0)
                        self._tm_last = (b_ == 3)
                        self.proj_tm(ws, bank, slice(b_ * 128, (b_ + 1) * 128), slice(t0, t0 + dil * 127 + 1, dil))
                    P.op("act", lambda h: h.activation(
                        out=self.yT_t[:, 6 + g, blk4 * 512:(blk4 + 1) * 512], in_=bank.t[:], func=AF.Copy),
                        reads=[bank], writes=[self.yT[6 + g]])
            ws = self.load_w(d["win"].t[60 + j, :, :])
            sgb = self.wk("att_sgb", [128, T], BF16, n=1)
            for tg in range(4):
                bank = self.banks[tg % 2]
                self.proj_fm(ws, tg, bank)
                P.op("act", lambda h: h.activation(
                    out=sgb.t[:, tg * 512:(tg + 1) * 512], in_=bank.t[:], func=AF.Silu),
                    reads=[bank], writes=[sgb])
            for R in range(4):
                ub = self.banks[4 + 2 * (R % 2)]
                db = self.banks[5 + 2 * (R % 2)]
                units = []
                for n in range(4 * R, 4 * R + 4):
                    qs = slice(128 * n, 128 * n + 128)
                    kb = []
                    if n >= 1:
                        kb.append((slice(128 * (n - 1), 128 * n), n - 1, 0))
                    kb.append((qs, n, 1))
                    units.append((0, qs, slice((n - 4 * R) * 128, (n - 4 * R) * 128 + 128), 128, kb, 0))
                for r in range(4):
                    q0 = r + 4 * 128 * R
                    qs = slice(q0, q0 + 4 * 127 + 1, 4)
                    kb = []
                    if R >= 1:
                        k0 = r + 4 * 128 * (R - 1)
                        kb.append((slice(k0, k0 + 4 * 127 + 1, 4), r * 4 + R - 1, 0))
                    kb.append((qs, r * 4 + R, 1))
                    units.append((1, qs, slice(r, r + 4 * 127 + 1, 4), 128, kb, 0))
                for r in range(16):
                    q0 = r + 16 * 32 * R
                    qs = slice(q0, q0 + 16 * 31 + 1, 16)
                    kb = [(slice(r, r + 16 * 127 + 1, 16), r, 1)]
                    units.append((2, qs, slice(r, r + 16 * 31 + 1, 16), 32, kb, 32 * R))
                nun = len(units)

                def emit_s(ui):
                    g, qs, osl, nq, kb, moff0 = units[ui]
                    st = self.banks[2 + ui % 2]
                    nkb = len(kb)
                    for b_, (ks, vblk, cur) in enumerate(kb):
                        P.op("pe", lambda h: h.matmul(
                            st.t[:, b_ * nq:(b_ + 1) * nq], self.yT_t[:, 3 + g, ks], self.yT_t[:, g, qs],
                            start=(b_ == 0), stop=(b_ == nkb - 1)),
                            reads=[self.yT[3 + g], self.yT[g]], writes=[st])

                def emit_rest(ui):
                    g, qs, osl, nq, kb, moff0 = units[ui]
                    st = self.banks[2 + ui % 2]
                    nkb = len(kb)
                    nk = nkb * nq
                    ex = self.wk("att_ex", [128, 256], F32, n=3)
                    P.op("act", lambda h: h.activation(
                        out=ex.t[:, 0:nk], in_=st.t[:, 0:nk], func=AF.Exp, scale=scale),
                        reads=[st], writes=[ex])
                    pt = self.wk("att_pt", [128, 256], BF16, n=3)
                    for b_, (ks, vblk, cur) in enumerate(kb):
                        mo = g * 256 + cur * 128 + moff0
                        P.op("dve", lambda h: h.tensor_tensor(
                            out=pt.t[:, b_ * nq:(b_ + 1) * nq], in0=ex.t[:, b_ * nq:(b_ + 1) * nq],
                            in1=am.t[:, mo:mo + nq], op=ALU.mult),
                            reads=[ex, am], writes=[pt])
                    return pt

                def emit_ud(ui, pt):
                    g, qs, osl, nq, kb, moff0 = units[ui]
                    nkb = len(kb)
                    for b_, (ks, vblk, cur) in enumerate(kb):
                        first = (ui == 0 and b_ == 0)
                        lastm = (ui == nun - 1 and b_ == nkb - 1)
                        P.op("pe", lambda h: h.matmul(
                            ub.t[:, osl], self.yT_t[:, 6 + g, vblk * 128:(vblk + 1) * 128],
                            pt.t[:, b_ * nq:(b_ + 1) * nq], start=first, stop=lastm),
                            reads=[self.yT[6 + g], pt], writes=[ub])
                        P.op("pe", lambda h: h.matmul(
                            db.t[:, osl], self.ones.t[:], pt.t[:, b_ * nq:(b_ + 1) * nq],
                            start=first, stop=lastm), reads=[self.ones, pt], writes=[db])

                emit_s(0)
                for ui in range(nun):
                    if ui + 1 < nun:
                        emit_s(ui + 1)
                    pt = emit_rest(ui)
                    emit_ud(ui, pt)
                rec = self.wk("att_rec", [128, 512], F32)
                P.op("dve", lambda h: h.reciprocal(out=rec.t[:], in_=db.t[:]), reads=[db], writes=[rec])
                P.op("dve", lambda h: h.tensor_tensor(out=rec.t[:], in0=ub.t[:], in1=rec.t[:], op=ALU.mult),
                     reads=[ub], writes=[rec])
                P.op("pool", lambda h: h.tensor_tensor(
                    out=self.yT_t[:, 12 + j, R * 512:(R + 1) * 512], in0=rec.t[:], in1=sgb.t[:, R * 512:(R + 1) * 512],
                    op=ALU.mult), reads=[rec, sgb], writes=[self.yT[12 + j]])

    def odd_layer(self, li):
        self.gla(li)
        self.end_phase()
        self.sgu(li)
        self.end_phase()

    def gla(self, li):
        P = self.P
        d = self.Ld[li]
        sp = self.spar
        C8, C9, C10, C11, C13 = 8, 9, 10, 11, 13
        yf = self.yflat
        cst = self.cst
        triT = cst.t[:, 0:128]
        uT = cst.t[:, 128:256]
        glam = cst.t[:, 256:384]
        aT = self.palloc("gla_aT", [17, T], F32)
        wal = self.palloc("gla_wal", [17, 512], F32)
        wina = self.palloc("gla_wina", [128, 256], BF16)
        P.dma("sp", wal.t[:], d["wal"].t[:, :], wal, wal)
        P.dma("pool", wina.t[:], d["wina"].t[:, :], wina, wina)
        P.op("dve", lambda h: h.memset(aT.t[:], 1.0), writes=[aT])
        for tg in range(4):
            bank = self.banks[tg % 2]
            for kc in range(NCH):
                P.op("pe", lambda h: h.matmul(
                    bank.t[0:16, :], wina.t[:, kc * 16:(kc + 1) * 16], self.hT_t[:, kc, tg * 512:(tg + 1) * 512],
                    start=(kc == 0), stop=(kc == NCH - 1)), reads=[wina, self.hT[kc]], writes=[bank])
            P.op("act", lambda h: h.activation(out=aT.t[0:16, tg * 512:(tg + 1) * 512], in_=bank.t[0:16, :],
                                               func=AF.Copy), reads=[bank], writes=[aT])
        S = self.palloc("gla_S", [128, 256], F32)
        Sb = [self.palloc(f"gla_Sb{i}", [128, 256], BF16) for i in range(2)]
        dec = self.palloc("gla_dec", [128, 32], F32)
        for hd in range(4):
            wq = self.load_w(d["win"].t[0 + hd, :, :])
            wk_ = self.load_w(d["win"].t[4 + hd, :, :])
            wv0 = self.load_w(d["win"].t[8 + 2 * hd, :, :])
            wv1 = self.load_w(d["win"].t[9 + 2 * hd, :, :])
            wgc = [self.load_w(d["win"].t[16 + 2 * hd + vc, :, :]) for vc in range(2)]
            P.op("dve", lambda h: h.memset(S.t[:], 0.0), writes=[S])
            P.op("dve", lambda h: h.memset(Sb[0].t[:], 0.0), writes=[Sb[0]])
            for tg in range(4):
                sl = slice(tg * 512, (tg + 1) * 512)
                b2 = self.banks[2]
                for ti in range(4):
                    tk0 = tg * 512 + ti * 128
                    P.op("pe", lambda h: h.matmul(
                        b2.t[:, ti * 128:(ti + 1) * 128], aT.t[0:17, tk0:tk0 + 128], wal.t[0:17, hd * 128:(hd + 1) * 128],
                        start=(ti == 0), stop=(ti == 3)), reads=[aT, wal], writes=[b2])
                L = self.wk("gla_L", [128, 512], F32)
                P.op("act", lambda h: h.activation(out=L.t[:], in_=b2.t[:], func=AF.Exp, scale=-1.0),
                     reads=[b2], writes=[L])
                P.op("act", lambda h: h.activation(out=L.t[:], in_=L.t[:], func=AF.Ln, bias=self.oneb.t[:, 0:1]),
                     reads=[self.oneb], writes=[L])
                b3 = self.banks[3]
                for ti in range(4):
                    P.op("pe", lambda h: h.matmul(
                        b3.t[:, ti * 128:(ti + 1) * 128], L.t[:, ti * 128:(ti + 1) * 128], triT,
                        start=(ti == 0), stop=(ti == 3)), reads=[L, cst], writes=[b3])
                Eb = self.wk("gla_Eb", [128, 512], F32)
                Enb = self.wk("gla_Enb", [128, 512], F32)
                P.op("act", lambda h: h.activation(out=Eb.t[:], in_=b3.t[:], func=AF.Exp), reads=[b3], writes=[Eb])
                P.op("act", lambda h: h.activation(out=Enb.t[:], in_=b3.t[:], func=AF.Exp, scale=-1.0),
                     reads=[b3], writes=[Enb])
                P.op("dve", lambda h: h.tensor_copy(out=dec.t[:, tg * 8:(tg + 1) * 8], in_=Eb.t[:, 63:512:64]),
                     reads=[Eb], writes=[dec])
                for ti in range(4):
                    P.op("pe", lambda h: h.matmul(
                        b2.t[:, ti * 128:(ti + 1) * 128], uT, L.t[:, ti * 128:(ti + 1) * 128],
                        start=(ti == 0), stop=(ti == 3)), reads=[L, cst], writes=[b2])
                Ed = self.wk("gla_Ed", [128, 512], F32)
                P.op("act", lambda h: h.activation(out=Ed.t[:], in_=b2.t[:], func=AF.Exp), reads=[b2], writes=[Ed])
                bq = self.banks[0]
                self.proj_fm(wq, tg, bq)
                P.op("dve", lambda h: h.scalar_tensor_tensor(
                    out=self.yT_t[:, C8, sl], in0=bq.t[:], scalar=128.0 ** -0.5, in1=Eb.t[:],
                    op0=ALU.mult, op1=ALU.mult), reads=[bq, Eb], writes=[self.yT[C8]])
                bk = self.banks[1]
                self.proj_fm(wk_, tg, bk)
                P.op("dve", lambda h: h.tensor_tensor(
                    out=self.yT_t[:, C9, sl], in0=bk.t[:], in1=Enb.t[:], op=ALU.mult),
                    reads=[bk, Enb], writes=[self.yT[C9]])
                bkt = self.banks[0]
                for ti in range(4):
                    tk0 = tg * 512 + ti * 128
                    self._tm_first = (ti == 0)
                    self._tm_last = (ti == 3)
                    self.proj_tm(wk_, bkt, slice(ti * 128, (ti + 1) * 128), slice(tk0, tk0 + 128))
                P.op("dve", lambda h: h.tensor_tensor(
                    out=self.yT_t[:, C10, sl], in0=bkt.t[:], in1=Ed.t[:], op=ALU.mult),
                    reads=[bkt, Ed], writes=[self.yT[C10]])
                for half in range(2):
                    bv = self.banks[1] if half == 0 else self.banks[0]
                    for t2 in range(2):
                        ti = half * 2 + t2
                        tk0 = tg * 512 + ti * 128
                        for vc, wv in enumerate((wv0, wv1)):
                            self._tm_first = (t2 == 0 and vc == 0)
                            self._tm_last = (t2 == 1 and vc == 1)
                            self.proj_tm(wv, bv, slice(t2 * 256 + vc * 128, t2 * 256 + (vc + 1) * 128),
                                         slice(tk0, tk0 + 128))
                    o0 = C11 * T + (tg * 4 + half * 2) * 256
                    P.op("act", lambda h: h.activation(out=yf[:, o0:o0 + 512], in_=bv.t[:], func=AF.Copy),
                         reads=[bv], writes=[self.yT[C11], self.yT[C11 + 1]])
                for vc in range(2):
                    bg = self.banks[vc]
                    self.proj_fm(wgc[vc], tg, bg)
                    P.op("act", lambda h: h.activation(out=self.yT_t[:, C13 + vc, sl], in_=bg.t[:], func=AF.Silu),
                         reads=[bg], writes=[self.yT[C13 + vc]])
                ob = [self.banks[4], self.banks[5]]
                for ti in range(4):
                    tile = tg * 4 + ti
                    tk0 = tile * 128
                    b6 = self.banks[6]
                    P.op("pe", lambda h: h.matmul(b6.t[:, 0:128], self.yT_t[:, C9, tk0:tk0 + 128],
                                                  self.yT_t[:, C8, tk0:tk0 + 128], start=True, stop=True),
                         reads=[self.yT[C8], self.yT[C9]], writes=[b6])
                    attm = self.wk("gla_attm", [128, 128], BF16)
                    P.op("dve", lambda h: h.tensor_tensor(out=attm.t[:], in0=b6.t[:, 0:128], in1=glam, op=ALU.mult),
                         reads=[b6, cst], writes=[attm])
                    for half in range(2):
                        c = tile * 2 + half
                        q0 = c * 64
                        sbc = Sb[c % 2]
                        sbn = Sb[(c + 1) % 2]
                        for vc in range(2):
                            P.op("pe", lambda h: h.matmul(
                                ob[vc].t[:, ti * 128 + half * 64: ti * 128 + half * 64 + 64],
                                sbc.t[:, vc * 128:(vc + 1) * 128], self.yT_t[:, C8, q0:q0 + 64],
                                start=(ti == 0 and half == 0), stop=False),
                                reads=[sbc, self.yT[C8]], writes=[ob[vc]])
                        b7 = self.banks[7]
                        kd0 = C10 * T + tile * 128
                        v0 = C11 * T + tile * 256
                        ps_ = slice(half * 64, (half + 1) * 64)
                        P.op("pe", lambda h: h.matmul(
                            b7.t[:, 0:256], yf[ps_, kd0:kd0 + 128], yf[ps_, v0:v0 + 256], start=True, stop=True),
                            reads=[self.yT[C10], self.yT[C11], self.yT[C11 + 1]], writes=[b7])
                        P.op("dve", lambda h: h.scalar_tensor_tensor(
                            out=S.t[:], in0=S.t[:], scalar=dec.t[:, c:c + 1], in1=b7.t[:, 0:256],
                            op0=ALU.mult, op1=ALU.add), reads=[dec, b7], writes=[S])
                        P.op("act", lambda h: h.activation(out=sbn.t[:], in_=S.t[:], func=AF.Copy),
                             reads=[S], writes=[sbn])
                    for vc in range(2):
                        vv = C11 * T + tile * 256 + vc * 128
                        P.op("pe", lambda h: h.matmul(
                            ob[vc].t[:, ti * 128:(ti + 1) * 128], yf[:, vv:vv + 128], attm.t[:],
                            start=False, stop=(ti == 3)),
                            reads=[self.yT[C11], self.yT[C11 + 1], attm], writes=[ob[vc]])
                b3 = self.banks[3]
                for vc in range(2):
                    sq = self.wk("gla_sq", [128, 512], BF16)
                    P.op("act", lambda h: h.activation(out=sq.t[:], in_=ob[vc].t[:], func=AF.Square),
                         reads=[ob[vc]], writes=[sq])
                    P.op("pe", lambda h: h.matmul(b3.t[:], self.ones.t[:], sq.t[:], start=(vc == 0), stop=(vc == 1)),
                         reads=[self.ones, sq], writes=[b3])
                rs = self.wk("gla_rs", [128, 512], F32)
                P.op("act", lambda h: h.activation(out=rs.t[:], in_=b3.t[:], func=AF.Sqrt, scale=1.0 / 256.0,
                                                   bias=self.epsb.t[:, 0:1]), reads=[b3, self.epsb], writes=[rs])
                P.op("dve", lambda h: h.reciprocal(out=rs.t[:], in_=rs.t[:]), reads=[], writes=[rs])
                for vc in range(2):
                    t1 = self.wk("gla_t1", [128, 512], F32)
                    hn = sp.t[:, 16 + hd * 2 + vc:16 + hd * 2 + vc + 1]
                    P.op("dve", lambda h: h.scalar_tensor_tensor(
                        out=t1.t[:], in0=ob[vc].t[:], scalar=hn, in1=rs.t[:], op0=ALU.mult, op1=ALU.mult),
                        reads=[ob[vc], sp, rs], writes=[t1])
                    P.op("pool", lambda h: h.tensor_tensor(
                        out=self.yT_t[:, 2 * hd + vc, sl], in0=t1.t[:], in1=self.yT_t[:, C13 + vc, sl], op=ALU.mult),
                        reads=[t1, self.yT[C13 + vc]], writes=[self.yT[2 * hd + vc]])

    def sgu(self, li):
        P = self.P
        d = self.Ld[li]
        sp = self.spar
        cst = self.cst
        wsf = self.palloc("sgu_wsf", [128, 512], F32)
        wm = self.palloc("sgu_wm", [128, 512], BF16)
        bsb = self.palloc("sgu_bsb", [128, 512], F32)
        bsr = self.palloc("sgu_bsr", [128, 512], F32)
        P.dma("sp", wsf.t[:], d["wsT"].t[:, :], wsf, wsf)
        P.dma("sp", bsb.t[:], d["bsb"].t[:, :], bsb, bsb)
        for g in range(4):
            P.op("dve", lambda h: h.tensor_tensor(out=wm.t[:, g * 128:(g + 1) * 128], in0=wsf.t[:, g * 128:(g + 1) * 128],
                                                  in1=cst.t[:, 384:512], op=ALU.mult), reads=[wsf, cst], writes=[wm])
        m = self.palloc("sgu_m", [128, 2, T], BF16)
        vn = self.palloc("sgu_vn", [128, 16, 256], BF16)
        for g in range(4):
            for ti in range(4):
                P.op("dve", lambda h: h.tensor_copy(out=bsr.t[:, ti * 128:(ti + 1) * 128],
                                                    in_=bsb.t[:, g * 128:(g + 1) * 128]), reads=[bsb], writes=[bsr])
            for ec in range(2):
                wu = self.load_w(d["win"].t[24 + 2 * g + ec, :, :])
                wgd = self.load_w(d["win"].t[40 + 2 * g + ec, :, :])
                for tg in range(4):
                    sl = slice(tg * 512, (tg + 1) * 512)
                    b0, b1 = self.banks[0], self.banks[1]
                    self.proj_fm(wu, tg, b0)
                    self.proj_fm(wgd, tg, b1)
                    gu = self.wk("sgu_gu", [128, 512], F32)
                    sg = self.wk("sgu_sg", [128, 512], F32)
                    P.op("act", lambda h: h.activation(out=gu.t[:], in_=b0.t[:], func=AF.Gelu_apprx_tanh),
                         reads=[b0], writes=[gu])
                    P.op("act", lambda h: h.activation(out=sg.t[:], in_=b1.t[:], func=AF.Silu),
                         reads=[b1], writes=[sg])
                    P.op("pool", lambda h: h.tensor_tensor(out=m.t[:, ec, sl], in0=gu.t[:], in1=sg.t[:], op=ALU.mult),
                         reads=[gu, sg], writes=[m])
            wv = [self.load_w(d["win"].t[32 + 2 * g + ec, :, :]) for ec in range(2)]
            for tile in range(16):
                bank = self.banks[2 + tile % 2]
                tk0 = tile * 128
                for ec in range(2):
                    self._tm_first = (ec == 0)
                    self._tm_last = (ec == 1)
                    self.proj_tm(wv[ec], bank, slice(ec * 128, (ec + 1) * 128), slice(tk0, tk0 + 128))
                gv = self.wk("sgu_gv", [128, 256], F32)
                P.op("act", lambda h: h.activation(out=gv.t[:], in_=bank.t[:, 0:256], func=AF.Gelu_apprx_tanh),
                     reads=[bank], writes=[gv])
                st6 = self.wk("sgu_st6", [128, 6], F32)
                mv = self.wk("sgu_mv", [128, 2], F32)
                P.op("dve", lambda h: h.bn_stats(out=st6.t[:], in_=gv.t[:]), reads=[gv], writes=[st6])
                P.op("dve", lambda h: h.bn_aggr(out=mv.t[:], in_=st6.t[:]), reads=[st6], writes=[mv])
                sd = self.wk("sgu_sd", [128, 1], F32)
                P.op("act", lambda h: h.activation(out=sd.t[:], in_=mv.t[:, 1:2], func=AF.Sqrt,
                                                   bias=self.epsb.t[:, 0:1]), reads=[mv, self.epsb], writes=[sd])
                P.op("dve", lambda h: h.reciprocal(out=sd.t[:], in_=sd.t[:]), reads=[], writes=[sd])
                P.op("dve", lambda h: h.tensor_scalar(out=vn.t[:, tile, :], in0=gv.t[:], scalar1=mv.t[:, 0:1],
                                                      scalar2=sd.t[:, 0:1], op0=ALU.subtract, op1=ALU.mult),
                     reads=[gv, mv, sd], writes=[vn])
            for ec in range(2):
                for tg in range(4):
                    sl = slice(tg * 512, (tg + 1) * 512)
                    bank = self.banks[4 + (ec * 4 + tg) % 2]
                    for ti in range(4):
                        tile = tg * 4 + ti
                        P.op("pe", lambda h: h.matmul(
                            bank.t[:, ti * 128:(ti + 1) * 128], vn.t[:, tile, ec * 128:(ec + 1) * 128],
                            wm.t[:, g * 128:(g + 1) * 128], start=(ti == 0), stop=(ti == 3)),
                            reads=[vn, wm], writes=[bank])
                    t1 = self.wk("sgu_t1", [128, 512], F32)
                    vnm = sp.t[:, 24 + g * 2 + ec:24 + g * 2 + ec + 1]
                    P.op("dve", lambda h: h.scalar_tensor_tensor(
                        out=t1.t[:], in0=bank.t[:], scalar=vnm, in1=bsr.t[:], op0=ALU.mult, op1=ALU.add),
                        reads=[bank, sp, bsr], writes=[t1])
                    P.op("pool", lambda h: h.tensor_tensor(
                        out=self.yT_t[:, 8 + 2 * g + ec, sl], in0=t1.t[:], in1=m.t[:, ec, sl], op=ALU.mult),
                        reads=[t1, m], writes=[self.yT[8 + 2 * g + ec]])

    def build(self):
        nl = len(self.layers)
        self.load_params(0)
        self.first_norm()
        for li, kind in enumerate(self.layers):
            if kind == "e":
                self.even_layer(li)
            else:
                self.odd_layer(li)
            last = (li == nl - 1)
            if not last:
                pass
            self.out_phase(li, last)
            if not last:
                self.load_params(li + 1)
        return self.nc


def _tile_w(w, ncol_chunks=None):
    K, C = w.shape
    cc = C // 128
    a = w.reshape(K // 128, 128, cc, 128)
    a = np.ascontiguousarray(a.transpose(2, 1, 0, 3))
    return a.reshape(cc, 128, (K // 128) * 128)


def _pp(v):
    return np.ascontiguousarray(v.reshape(-1, 128).T)


def _consts():
    s = np.arange(128)[:, None]
    t = np.arange(128)[None, :]
    same = (s // 64) == (t // 64)
    triT = np.where(same & (s <= t), -1.0 / 16.0, 0.0)
    uT = np.where(same & (s > t), -1.0 / 16.0, 0.0)
    glam = np.where(same & (s <= t), 1.0, 0.0)
    sgm = np.where(s <= t, 1.0, 0.0)
    cst = np.concatenate([triT, uT, glam, sgm], axis=1).astype(np.float32)
    am = np.zeros((4, 128, 3, 256), np.float64)
    k = np.arange(128)[:, None].astype(np.float64)
    q = np.arange(128)[None, :].astype(np.float64)
    for g in range(3):
        for j in range(4):
            slope = 2.0 ** (-8.0 * (g + 3 * j + 1) / 12.0)
            dil = DIL[g]
            prev = np.where(k >= q, np.exp(-slope * dil * (q + 128 - k)), 0.0)
            cur = np.where(k <= q, np.exp(-slope * dil * (q - k)), 0.0)
            am[j, :, g, 0:128] = prev
            am[j, :, g, 128:256] = cur
    return cst, am.reshape(4, 128, 768).astype(np.float32)


LAYERS = ["e", "o", "e", "o"]


def prep_shared(inp, layers):
    f = lambda a: np.ascontiguousarray(np.asarray(a, dtype=np.float32))
    m = {}
    cst, am = _consts()
    m["cst"] = cst
    m["amask"] = am
    m["fg"] = _pp(f(inp["final_norm"]))
    ie = io = 0
    for li, kind in enumerate(layers):
        if kind == "e":
            i = ie
            ie += 1
            m[f"L{li}_win"] = _tile_w(f(inp["ev_w_in"][i]))
            m[f"L{li}_wout"] = _tile_w(f(inp["ev_w_out"][i]))
            sp = np.zeros((128, 112), np.float32)
            sp[:, 0:16] = _pp(f(inp["ev_norm"][i]))
            cw = f(inp["ev_conv_w"][i])
            sp[:, 16:64] = cw.reshape(4, 12, 128).transpose(2, 1, 0).reshape(128, 48)
            sp[:, 64:76] = _pp(f(inp["ev_conv_b"][i]))
            sp[:, 76:88] = _pp(f(inp["ev_lam"][i]))
            sp[:, 88:100] = _pp(f(inp["ev_b_a"][i]).reshape(-1))
            sp[:, 100:112] = _pp(f(inp["ev_b_i"][i]).reshape(-1))
            m[f"L{li}_sp"] = sp
            wa = f(inp["ev_w_a"][i]).transpose(1, 0, 2).reshape(128, 1536)
            wi = f(inp["ev_w_i"][i]).transpose(1, 0, 2).reshape(128, 1536)
            m[f"L{li}_wg"] = np.ascontiguousarray(np.concatenate([wa, wi], axis=1))
        else:
            i = io
            io += 1
            w = f(inp["od_w_in"][i])
            cols = np.concatenate([np.arange(0, 2048), np.arange(2064, 6160)])
            m[f"L{li}_win"] = _tile_w(np.ascontiguousarray(w[:, cols]))
            wa = w[:, 2048:2064].reshape(16, 128, 16).transpose(1, 0, 2).reshape(128, 256)
            m[f"L{li}_wina"] = np.ascontiguousarray(wa)
            m[f"L{li}_wout"] = _tile_w(f(inp["od_w_out"][i]))
            sp = np.zeros((128, 32), np.float32)
            sp[:, 0:16] = _pp(f(inp["od_norm"][i]))
            sp[:, 16:24] = _pp(f(inp["od_head_norm"][i]).reshape(-1))
            sp[:, 24:32] = _pp(f(inp["od_v_norm"][i]).reshape(-1))
            m[f"L{li}_sp"] = sp
            m[f"L{li}_wal"] = np.ascontiguousarray(
                np.concatenate([f(inp["od_w_alpha"][i]), f(inp["od_b_alpha"][i])[None, :]], axis=0))
            ws = f(inp["od_w_s"][i])
            m[f"L{li}_wsT"] = np.ascontiguousarray(ws.transpose(2, 0, 1).reshape(128, 512))
            bs = f(inp["od_b_s"][i]).reshape(1, 512)
            m[f"L{li}_bsb"] = np.ascontiguousarray(np.broadcast_to(bs, (128, 512)))
    return m


def run_layers(x, inp, layers, final=True):
    b = Builder(layers, final=final)
    nc = b.build()
    shared = prep_shared(inp, layers)
    in_maps = []
    for core in range(N_CORES):
        bi = core % 4
        xt = np.ascontiguousarray(x[bi].T).reshape(NCH, 128, T)
        mm = dict(shared)
        mm["xin"] = xt
        in_maps.append(mm)
    res = run_bass_kernel_spmd(nc, in_maps, core_ids=list(range(N_CORES)))
    out = np.empty((4, T, D), np.float32)
    for bi in range(4):
        out[bi] = res.results[bi]["yout"].reshape(D, T).T
    return out


def kernel(**inputs):
    x = np.asarray(inputs["x"], dtype=np.float32)
    return run_layers(x, inputs, LAYERS, final=True)
```
